# Optimizing a Trainium2 kernel written in Bass

```python
import jax
import jax.numpy as jnp
from jax import lax
import numpy as np

D_MODEL = 2048
BATCH = 2
SEQ = 8192
DEPTH = 4

MLA_HEADS = 8
MLA_Q_LORA = 512
MLA_KV_LORA = 512
MLA_NOPE_DIM = 128
MLA_ROPE_DIM = 64
MLA_V_DIM = 128
MLA_QK_DIM = MLA_NOPE_DIM + MLA_ROPE_DIM
ROPE_THETA = 10000.0
Q_BLOCK = 128

GDN_HEADS = 8
GDN_K_DIM = 128
GDN_V_DIM = 128
GDN_CONV = 4
GDN_CHUNK = 64

IN_SPLITS = (
    MLA_Q_LORA,
    MLA_KV_LORA,
    MLA_ROPE_DIM,
    GDN_HEADS * GDN_K_DIM,
    GDN_HEADS * GDN_K_DIM,
    GDN_HEADS * GDN_V_DIM,
    GDN_HEADS * GDN_V_DIM,
    GDN_HEADS,
    GDN_HEADS,
)
IN_COLS = sum(IN_SPLITS)
MIX_WIDTH = MLA_HEADS * MLA_V_DIM + GDN_HEADS * GDN_V_DIM

RWKV_HEAD = 64
RWKV_HEADS = D_MODEL // RWKV_HEAD
RWKV_DECAY_LORA = max(32, round(1.8 * D_MODEL ** 0.5 / 32) * 32)
RWKV_AAA_LORA = max(32, round(1.8 * D_MODEL ** 0.5 / 32) * 32)
RWKV_MV_LORA = max(32, round(1.3 * D_MODEL ** 0.5 / 32) * 32)
RWKV_GATE_LORA = max(32, round(0.6 * D_MODEL ** 0.8 / 32) * 32)
RWKV_LN_EPS = 64e-5

FFN_HIDDEN = -(-8 * D_MODEL // (3 * 256)) * 256
RMS_EPS = 1e-6

kernel_name = "hybrid_mla_gdn_rwkv7_sandwich"


def rms_norm(x, w, eps=RMS_EPS):
    xf = x.astype(jnp.float32)
    y = xf * lax.rsqrt(jnp.mean(xf * xf, axis=-1, keepdims=True) + eps)
    return (y * w.astype(jnp.float32)).astype(x.dtype)


def l2_normalize(x, eps=1e-6):
    xf = x.astype(jnp.float32)
    return xf * lax.rsqrt(jnp.sum(xf * xf, axis=-1, keepdims=True) + eps)


def rope_tables(positions, dtype):
    inv_freq = 1.0 / (ROPE_THETA ** (jnp.arange(0, MLA_ROPE_DIM, 2, dtype=jnp.float32) / MLA_ROPE_DIM))
    ang = positions.astype(jnp.float32)[..., None] * inv_freq
    return jnp.cos(ang).astype(dtype), jnp.sin(ang).astype(dtype)


def apply_rope(x, cos, sin):
    x1, x2 = jnp.split(x, 2, axis=-1)
    return jnp.concatenate([x1 * cos - x2 * sin, x2 * cos + x1 * sin], axis=-1)


def causal_depthwise_conv(x, w):
    k_width, ch = w.shape
    return lax.conv_general_dilated(
        x, w[:, None, :].astype(x.dtype), window_strides=(1,), padding=[(k_width - 1, 0)],
        dimension_numbers=("NWC", "WIO", "NWC"), feature_group_count=ch)


def mla_causal_attention(q_nope, q_rope, k_nope, k_rope, v):
    b, s, h, dn = q_nope.shape
    nb = s // Q_BLOCK
    scale = MLA_QK_DIM ** -0.5
    qn = jnp.moveaxis(q_nope.reshape(b, nb, Q_BLOCK, h, dn), 1, 0)
    qr = jnp.moveaxis(q_rope.reshape(b, nb, Q_BLOCK, h, MLA_ROPE_DIM), 1, 0)
    k_pos = jnp.arange(s)

    def one_block(args):
        i, qn_b, qr_b = args
        sc = (jnp.einsum("bqhd,bkhd->bhqk", qn_b, k_nope)
              + jnp.einsum("bqhr,bkr->bhqk", qr_b, k_rope)).astype(jnp.float32) * scale
        q_pos = i * Q_BLOCK + jnp.arange(Q_BLOCK)
        sc = jnp.where(k_pos[None, :] <= q_pos[:, None], sc, -jnp.inf)
        p = jax.nn.softmax(sc, axis=-1).astype(v.dtype)
        return jnp.einsum("bhqk,bkhd->bqhd", p, v)

    out = lax.map(one_block, (jnp.arange(nb), qn, qr))
    return jnp.moveaxis(out, 0, 1).reshape(b, s, h, v.shape[-1])


def gated_delta_rule_chunked(q, k, v, g, beta):
    b, s, h, dk = q.shape
    dv = v.shape[-1]
    c = GDN_CHUNK
    nc = s // c

    def to_chunks(t):
        return jnp.moveaxis(t.reshape((b, nc, c) + t.shape[2:]), 3, 1)

    q, k, v = to_chunks(q * dk ** -0.5), to_chunks(k), to_chunks(v)
    g, beta = to_chunks(g), to_chunks(beta)
    gam = jnp.cumsum(g, axis=-1)
    incl = jnp.tril(jnp.ones((c, c), dtype=bool))
    strict = jnp.tril(jnp.ones((c, c), dtype=bool), -1)
    decay = jnp.exp(jnp.where(incl, gam[..., :, None] - gam[..., None, :], -jnp.inf))
    kb = k * beta[..., None]
    vb = v * beta[..., None]
    a_mat = jnp.where(strict, jnp.einsum("bhnik,bhnjk->bhnij", kb, k) * decay, 0.0)
    rhs = jnp.concatenate([vb, kb * jnp.exp(gam)[..., None]], axis=-1)
    sol = lax.linalg.triangular_solve(a_mat + jnp.eye(c, dtype=a_mat.dtype), rhs,
                                      left_side=True, lower=True, unit_diagonal=True)
    u, w = sol[..., :dv], sol[..., dv:]
    attn = jnp.einsum("bhnik,bhnjk->bhnij", q, k) * decay
    q_dec = q * jnp.exp(gam)[..., None]
    k_dec = k * jnp.exp(gam[..., -1:] - gam)[..., None]
    last = jnp.exp(gam[..., -1])

    def step(state, inp):
        qd, at, u_c, w_c, kd, ld = inp
        e = u_c - jnp.einsum("bhck,bhkv->bhcv", w_c, state)
        o = jnp.einsum("bhck,bhkv->bhcv", qd, state) + jnp.einsum("bhij,bhjv->bhiv", at, e)
        state = state * ld[..., None, None] + jnp.einsum("bhck,bhcv->bhkv", kd, e)
        return state, o

    xs = tuple(jnp.moveaxis(t, 2, 0) for t in (q_dec, attn, u, w, k_dec, last))
    _, o = lax.scan(step, jnp.zeros((b, h, dk, dv), jnp.float32), xs)
    return o.transpose(1, 0, 3, 2, 4).reshape(b, s, h, dv)


def hybrid_mla_gdn_mixer(xn, positions, w_in, q_norm, w_uq, kv_norm, w_ukv,
                         conv_w, a_log, dt_bias, out_norm, w_out):
    b, s, _ = xn.shape
    dt = xn.dtype
    hcat = xn @ w_in
    offs = np.cumsum(IN_SPLITS)[:-1].tolist()
    c_q, c_kv, k_rope, gq, gk, gv, gz, gb, ga = jnp.split(hcat, offs, axis=-1)

    q = (rms_norm(c_q, q_norm) @ w_uq).reshape(b, s, MLA_HEADS, MLA_QK_DIM)
    kv = (rms_norm(c_kv, kv_norm) @ w_ukv).reshape(b, s, MLA_HEADS, MLA_NOPE_DIM + MLA_V_DIM)
    q_nope, q_rope = q[..., :MLA_NOPE_DIM], q[..., MLA_NOPE_DIM:]
    k_nope, v_mla = kv[..., :MLA_NOPE_DIM], kv[..., MLA_NOPE_DIM:]
    cos, sin = rope_tables(positions, dt)
    q_rope = apply_rope(q_rope, cos[:, :, None, :], sin[:, :, None, :])
    k_rope = apply_rope(k_rope, cos, sin)
    mla_out = mla_causal_attention(q_nope, q_rope, k_nope, k_rope, v_mla)
    mla_out = mla_out.reshape(b, s, MLA_HEADS * MLA_V_DIM)

    qkv = jax.nn.silu(causal_depthwise_conv(jnp.concatenate([gq, gk, gv], axis=-1), conv_w))
    hq, hk, hv = jnp.split(qkv, [GDN_HEADS * GDN_K_DIM, 2 * GDN_HEADS * GDN_K_DIM], axis=-1)
    hq = l2_normalize(hq.reshape(b, s, GDN_HEADS, GDN_K_DIM))
    hk = l2_normalize(hk.reshape(b, s, GDN_HEADS, GDN_K_DIM))
    hv = hv.reshape(b, s, GDN_HEADS, GDN_V_DIM).astype(jnp.float32)
    beta = jax.nn.sigmoid(gb.astype(jnp.float32))
    g = -jnp.exp(a_log.astype(jnp.float32)) * jax.nn.softplus(
        ga.astype(jnp.float32) + dt_bias.astype(jnp.float32))
    o = gated_delta_rule_chunked(hq, hk, hv, g, beta)
    z = gz.reshape(b, s, GDN_HEADS, GDN_V_DIM)
    gdn_out = (rms_norm(o, out_norm).astype(dt) * jax.nn.silu(z)).reshape(b, s, GDN_HEADS * GDN_V_DIM)

    return jnp.concatenate([mla_out, gdn_out], axis=-1) @ w_out


def rwkv7_recurrence(r, decay, k, v, kk, a):
    b, s, h, n = r.shape

    def step(state, inp):
        r_t, d_t, k_t, v_t, kk_t, a_t = inp
        sk = jnp.einsum("bhvk,bhk->bhv", state, kk_t)
        state = (state * d_t[:, :, None, :]
                 - sk[..., None] * (kk_t * a_t)[:, :, None, :]
                 + v_t[..., None] * k_t[:, :, None, :])
        return state, jnp.einsum("bhvk,bhk->bhv", state, r_t)

    xs = tuple(jnp.moveaxis(t, 1, 0) for t in (r, decay, k, v, kk, a))
    _, y = lax.scan(step, jnp.zeros((b, h, n, n), jnp.float32), xs)
    return jnp.moveaxis(y, 0, 1)


def rwkv7_time_mix(xn, v_first, mix, w_r, w_k, w_v, w_o, w0, w1, w2, a0, a1, a2,
                   g1, g2, k_k, k_a, r_k, ln_w, ln_b, vres):
    b, s, d = xn.shape
    dt = xn.dtype
    x_prev = jnp.pad(xn, ((0, 0), (1, 0), (0, 0)))[:, :-1]
    xx = x_prev - xn
    xr, xw, xk, xv, xa, xg = (xn + xx * mix[i] for i in range(6))

    r = xr @ w_r
    w_log = -jax.nn.softplus(-(w0 + jnp.tanh(xw @ w1) @ w2).astype(jnp.float32)) - 0.5
    k = xk @ w_k
    v = xv @ w_v
    if vres is None:
        v_first = v
    else:
        v0, v1, v2 = vres
        v = v + (v_first - v) * jax.nn.sigmoid(v0 + (xv @ v1) @ v2)
    a = jax.nn.sigmoid((a0 + (xa @ a1) @ a2).astype(jnp.float32))
    gate = jax.nn.sigmoid(xg @ g1) @ g2

    heads = lambda t: t.reshape(b, s, RWKV_HEADS, RWKV_HEAD).astype(jnp.float32)
    kk = l2_normalize(heads(k * k_k), eps=1e-12)
    a_h = heads(a)
    k_a_h = k_a.reshape(RWKV_HEADS, RWKV_HEAD).astype(jnp.float32)
    k_h = heads(k) * (1.0 + (a_h - 1.0) * k_a_h)
    r_h, v_h = heads(r), heads(v)
    decay = heads(jnp.exp(-jnp.exp(w_log)))
    y = rwkv7_recurrence(r_h, decay, k_h, v_h, kk, a_h)

    mu = jnp.mean(y, axis=-1, keepdims=True)
    var = jnp.mean(jnp.square(y - mu), axis=-1, keepdims=True)
    y = ((y - mu) * lax.rsqrt(var + RWKV_LN_EPS)).reshape(b, s, d)
    y = y * ln_w.astype(jnp.float32) + ln_b.astype(jnp.float32)
    bonus = jnp.sum(r_h * k_h * r_k.astype(jnp.float32), axis=-1, keepdims=True) * v_h
    y = (y + bonus.reshape(b, s, d)).astype(dt)
    return (y * gate) @ w_o, v_first


def swiglu(x, w_gate, w_up, w_down):
    return (jax.nn.silu(x @ w_gate) * (x @ w_up)) @ w_down


def setup_inputs(seed: int = 0) -> dict:
    key = jax.random.key(seed)
    ks = iter(jax.random.split(key, 64))
    ne, no = (DEPTH + 1) // 2, DEPTH // 2
    nv = max(no - 1, 0)
    d = D_MODEL

    def nrm(shape, scale):
        return scale * jax.random.normal(next(ks), shape, jnp.float32)

    def unif(shape, lo, hi):
        return jax.random.uniform(next(ks), shape, jnp.float32, lo, hi)

    def gain(shape):
        return 1.0 + nrm(shape, 0.02)

    x = jax.random.normal(next(ks), (BATCH, SEQ, d), jnp.float32)
    positions = jnp.broadcast_to(jnp.arange(SEQ, dtype=jnp.int32), (BATCH, SEQ))
    dt0 = jnp.exp(unif((ne, GDN_HEADS), float(np.log(1e-3)), float(np.log(1e-1))))
    return {
        "x": x,
        "positions": positions,
        "norm_mix_pre": gain((DEPTH, d)),
        "norm_mix_post": gain((DEPTH, d)),
        "norm_ffn_pre": gain((DEPTH, d)),
        "norm_ffn_post": gain((DEPTH, d)),
        "hyb_w_in": nrm((ne, d, IN_COLS), d ** -0.5),
        "mla_q_norm": gain((ne, MLA_Q_LORA)),
        "mla_w_uq": nrm((ne, MLA_Q_LORA, MLA_HEADS * MLA_QK_DIM), MLA_Q_LORA ** -0.5),
        "mla_kv_norm": gain((ne, MLA_KV_LORA)),
        "mla_w_ukv": nrm((ne, MLA_KV_LORA, MLA_HEADS * (MLA_NOPE_DIM + MLA_V_DIM)), MLA_KV_LORA ** -0.5),
        "gdn_conv_w": nrm((ne, GDN_CONV, GDN_HEADS * (2 * GDN_K_DIM + GDN_V_DIM)), GDN_CONV ** -0.5),
        "gdn_a_log": jnp.log(unif((ne, GDN_HEADS), 1.0, 16.0)),
        "gdn_dt_bias": dt0 + jnp.log(-jnp.expm1(-dt0)),
        "gdn_out_norm": gain((ne, GDN_V_DIM)),
        "hyb_w_out": nrm((ne, MIX_WIDTH, d), MIX_WIDTH ** -0.5),
        "rwkv_mix": unif((no, 6, d), 0.0, 1.0),
        "rwkv_w_r": nrm((no, d, d), d ** -0.5),
        "rwkv_w_k": nrm((no, d, d), d ** -0.5),
        "rwkv_w_v": nrm((no, d, d), d ** -0.5),
        "rwkv_w_o": nrm((no, d, d), d ** -0.5),
        "rwkv_w0": unif((no, d), -6.0, -1.0),
        "rwkv_w1": nrm((no, d, RWKV_DECAY_LORA), d ** -0.5),
        "rwkv_w2": nrm((no, RWKV_DECAY_LORA, d), 0.1 * RWKV_DECAY_LORA ** -0.5),
        "rwkv_a0": nrm((no, d), 0.1),
        "rwkv_a1": nrm((no, d, RWKV_AAA_LORA), d ** -0.5),
        "rwkv_a2": nrm((no, RWKV_AAA_LORA, d), 0.1 * RWKV_AAA_LORA ** -0.5),
        "rwkv_g1": nrm((no, d, RWKV_GATE_LORA), d ** -0.5),
        "rwkv_g2": nrm((no, RWKV_GATE_LORA, d), RWKV_GATE_LORA ** -0.5),
        "rwkv_k_k": 0.85 + nrm((no, d), 0.02),
        "rwkv_k_a": gain((no, d)),
        "rwkv_r_k": nrm((no, RWKV_HEADS, RWKV_HEAD), 0.1),
        "rwkv_ln_w": gain((no, d)),
        "rwkv_ln_b": nrm((no, d), 0.02),
        "rwkv_v0": 1.0 + nrm((nv, d), 0.1),
        "rwkv_v1": nrm((nv, d, RWKV_MV_LORA), d ** -0.5),
        "rwkv_v2": nrm((nv, RWKV_MV_LORA, d), 0.1 * RWKV_MV_LORA ** -0.5),
        "ffn_w_gate": nrm((DEPTH, d, FFN_HIDDEN), d ** -0.5),
        "ffn_w_up": nrm((DEPTH, d, FFN_HIDDEN), d ** -0.5),
        "ffn_w_down": nrm((DEPTH, FFN_HIDDEN, d), FFN_HIDDEN ** -0.5),
    }


def reference(x, positions, norm_mix_pre, norm_mix_post, norm_ffn_pre, norm_ffn_post,
              hyb_w_in, mla_q_norm, mla_w_uq, mla_kv_norm, mla_w_ukv, gdn_conv_w,
              gdn_a_log, gdn_dt_bias, gdn_out_norm, hyb_w_out,
              rwkv_mix, rwkv_w_r, rwkv_w_k, rwkv_w_v, rwkv_w_o, rwkv_w0, rwkv_w1, rwkv_w2,
              rwkv_a0, rwkv_a1, rwkv_a2, rwkv_g1, rwkv_g2, rwkv_k_k, rwkv_k_a, rwkv_r_k,
              rwkv_ln_w, rwkv_ln_b, rwkv_v0, rwkv_v1, rwkv_v2,
              ffn_w_gate, ffn_w_up, ffn_w_down):
    h = x
    v_first = None
    for layer in range(DEPTH):
        xn = rms_norm(h, norm_mix_pre[layer])
        if layer % 2 == 0:
            e = layer // 2
            mix_out = hybrid_mla_gdn_mixer(
                xn, positions, hyb_w_in[e], mla_q_norm[e], mla_w_uq[e], mla_kv_norm[e],
                mla_w_ukv[e], gdn_conv_w[e], gdn_a_log[e], gdn_dt_bias[e], gdn_out_norm[e],
                hyb_w_out[e])
        else:
            o = layer // 2
            vres = None if o == 0 else (rwkv_v0[o - 1], rwkv_v1[o - 1], rwkv_v2[o - 1])
            mix_out, v_first = rwkv7_time_mix(
                xn, v_first, rwkv_mix[o], rwkv_w_r[o], rwkv_w_k[o], rwkv_w_v[o], rwkv_w_o[o],
                rwkv_w0[o], rwkv_w1[o], rwkv_w2[o], rwkv_a0[o], rwkv_a1[o], rwkv_a2[o],
                rwkv_g1[o], rwkv_g2[o], rwkv_k_k[o], rwkv_k_a[o], rwkv_r_k[o],
                rwkv_ln_w[o], rwkv_ln_b[o], vres)
        h = h + rms_norm(mix_out, norm_mix_post[layer])
        f = swiglu(rms_norm(h, norm_ffn_pre[layer]), ffn_w_gate[layer], ffn_w_up[layer], ffn_w_down[layer])
        h = h + rms_norm(f, norm_ffn_post[layer])
    return h
```

```python
import contextlib
import math
import numpy as np
import concourse.bass as bass
import concourse.mybir as mybir
from concourse.bass_utils import run_bass_kernel_spmd


F32 = mybir.dt.float32
BF16 = mybir.dt.bfloat16
I32 = mybir.dt.int32
AF = mybir.ActivationFunctionType
ALU = mybir.AluOpType
AX = mybir.AxisListType

_WRITE_KEYS = ("out", "accum_out", "ap")


class _Ins:
    __slots__ = ("eng", "fn", "deps", "needed", "val", "sem", "is_dma", "idx")

    def __init__(self, eng, fn, is_dma=False):
        self.eng = eng
        self.fn = fn
        self.deps = []
        self.needed = False
        self.val = None
        self.sem = None
        self.is_dma = is_dma


class _Trk:
    __slots__ = ("last_w", "reads")

    def __init__(self):
        self.last_w = None
        self.reads = []


class Prog:
    ENGS = ("tensor", "vector", "scalar", "gpsimd", "sync")

    def __init__(self, nc):
        self.nc = nc
        self.streams = {e: [] for e in self.ENGS}
        self.trk = {}
        self.stack = contextlib.ExitStack()
        self.dma_sems = {}
        self.eng_sems = {}
        self.n_ins = 0

    def sb(self, name, shape, dt=F32):
        return self.stack.enter_context(self.nc.sbuf_tensor(name, list(shape), dt))

    def ps(self, name, shape, dt=F32):
        return self.stack.enter_context(self.nc.psum_tensor(name, list(shape), dt))

    def dram(self, name, shape, dt=F32, kind="Internal"):
        return self.nc.dram_tensor(name, list(shape), dt, kind=kind)

    def _t(self, ap):
        n = ap.tensor.name
        t = self.trk.get(n)
        if t is None:
            t = self.trk[n] = _Trk()
        return t

    def _record(self, ins, reads, writes):
        eng = ins.eng
        deps = []
        for ap in reads:
            t = self._t(ap)
            w = t.last_w
            if w is not None:
                if not (w.eng == eng and eng == "tensor" and not w.is_dma):
                    deps.append(w)
        for ap in writes:
            t = self._t(ap)
            w = t.last_w
            if w is not None and (w.is_dma or ins.is_dma or w.eng != eng or eng != "tensor"):
                if not (w.is_dma and ins.is_dma and w.sem == ins.sem):
                    deps.append(w)
            for r in t.reads:
                if r.is_dma or ins.is_dma or r.eng != eng or eng != "tensor":
                    deps.append(r)
        for ap in reads:
            self._t(ap).reads.append(ins)
        for ap in writes:
            t = self._t(ap)
            t.last_w = ins
            t.reads = []
        seen = set()
        for d in deps:
            if d is ins or id(d) in seen:
                continue
            seen.add(id(d))
            d.needed = True
            ins.deps.append(d)
        self.streams[eng].append(ins)
        self.n_ins += 1
        return ins

    def op(self, eng, name, *args, extra_reads=(), extra_writes=(), **kw):
        reads, writes = list(extra_reads), list(extra_writes)
        for k, v in kw.items():
            if isinstance(v, bass.AP):
                (writes if k in _WRITE_KEYS else reads).append(v)
        for i, v in enumerate(args):
            if isinstance(v, bass.AP):
                (writes if i == 0 else reads).append(v)
        if name == "matmul" and kw.get("start") is False:
            pass

        if eng == "gpsimd":
            for v in list(kw.values()) + list(args):
                if isinstance(v, bass.AP) and "psum" in str(v.space).lower():
                    raise ValueError("gpsimd cannot access PSUM: %s" % name)

        def fn(e, name=name, args=args, kw=kw):
            return getattr(e, name)(*args, **kw)

        return self._record(_Ins(eng, fn), reads, writes)

    def dma(self, eng, out, in_, key=None, **kw):
        if key is None:
            sbside = out if str(out.space).lower().find("dram") < 0 else in_
            key = sbside.tensor.name
        ins = _Ins(eng, lambda e, out=out, in_=in_, kw=kw: e.dma_start(out=out, in_=in_, **kw), is_dma=True)
        ins.sem = key
        ins.needed = True
        return self._record(ins, [in_], [out])

    def mm(self, out, lhsT, rhs, start=True, stop=True, **kw):
        return self.op("tensor", "matmul", out=out, lhsT=lhsT, rhs=rhs, start=start, stop=stop, **kw)

    def tr(self, out, in_, identity, **kw):
        return self.op("tensor", "transpose", out=out, in_=in_, identity=identity, **kw)

    def act(self, out, in_, func, eng="scalar", **kw):
        return self.op(eng, "activation", out=out, in_=in_, func=func, **kw)

    def tt(self, out, in0, in1, op, eng="vector"):
        return self.op(eng, "tensor_tensor", out=out, in0=in0, in1=in1, op=op)

    def ts(self, out, in0, scalar1, scalar2=None, op0=ALU.mult, op1=None, eng="vector", **kw):
        if op1 is None:
            return self.op(eng, "tensor_scalar", out=out, in0=in0, scalar1=scalar1, scalar2=scalar2, op0=op0, **kw)
        return self.op(eng, "tensor_scalar", out=out, in0=in0, scalar1=scalar1, scalar2=scalar2, op0=op0, op1=op1, **kw)

    def stt(self, out, in0, scalar, in1, op0, op1, eng="vector", **kw):
        return self.op(eng, "scalar_tensor_tensor", out=out, in0=in0, scalar=scalar, in1=in1, op0=op0, op1=op1, **kw)

    def copy(self, out, in_, eng="vector"):
        if eng == "scalar":
            return self.op("scalar", "copy", out=out, in_=in_)
        return self.op(eng, "tensor_copy", out=out, in_=in_)

    def memset(self, ap, val, eng="vector"):
        return self.op(eng, "memset", ap=ap, constant=val)

    def finish(self, final_waits=()):
        nc = self.nc
        for e in self.ENGS:
            self.eng_sems[e] = self.stack.enter_context(nc.semaphore("se_" + e))
        counts = {}
        for e in self.ENGS:
            c = 0
            for ins in self.streams[e]:
                if ins.is_dma:
                    k = ins.sem
                    if k not in self.dma_sems:
                        self.dma_sems[k] = self.stack.enter_context(nc.semaphore("sd_%d" % len(self.dma_sems)))
                    counts[k] = counts.get(k, 0) + 16
                    ins.val = counts[k]
                    ins.sem = self.dma_sems[k]
                elif ins.needed:
                    c += 1
                    ins.val = c
                    ins.sem = self.eng_sems[e]
        key_eng = {}
        for e in self.ENGS:
            for ins in self.streams[e]:
                if ins.is_dma:
                    ke = key_eng.setdefault(ins.sem.name, e)
                    assert ke == e, "DMA key shared across engines"
        self.n_waits = 0
        prog = self

        def replay(engname):
            def body(e):
                waited = {}
                for ins in prog.streams[engname]:
                    need = {}
                    for d in ins.deps:
                        nm = d.sem.name
                        if waited.get(nm, 0) < d.val and need.get(nm, (None, 0))[1] < d.val:
                            need[nm] = (d.sem, d.val)
                    for nm, (sem, val) in need.items():
                        e.wait_ge(sem, val)
                        waited[nm] = val
                        prog.n_waits += 1
                    r = ins.fn(e)
                    if ins.is_dma:
                        r.then_inc(ins.sem, 16)
                    elif ins.needed:
                        r.then_inc(ins.sem, 1)
                if engname == "sync":
                    need = {}
                    for d in final_waits:
                        nm = d.sem.name
                        if need.get(nm, (None, 0))[1] < d.val:
                            need[nm] = (d.sem, d.val)
                    for nm, (sem, val) in need.items():
                        e.wait_ge(sem, val)
            return body

        with nc.Block() as block:
            block.tensor(replay("tensor"))
            block.vector(replay("vector"))
            block.scalar(replay("scalar"))
            block.gpsimd(replay("gpsimd"))
            block.sync(replay("sync"))
        self.stack.close()


D = 2048
TB = 512
NKC = D // 128


class Ctx:
    def __init__(self, P, T=TB, wcols=256, wk=16):
        self.P = P
        self.T = T
        self.wcols = wcols
        self.wk = wk
        self.stage = [P.sb("wst%d" % i, [128, wk, wcols], F32) for i in range(2)]
        self.wbf = [P.sb("wbf%d" % i, [128, wk, wcols], BF16) for i in range(2)]
        self.pl = [P.ps("pl%d" % i, [128, 512], F32) for i in range(4)]
        self.wi = 0
        self.gi = 0
        self.cast_i = 0
        self.ones = P.sb("ones_bf", [128, 128], BF16)
        P.memset(self.ones[:], 1.0)


def linear(cx, W, kch, segs, rhs_fn, consume, T=None):
    P = cx.P
    T = T or cx.T
    groups = []
    cur = None
    for si, (c0, w) in enumerate(segs):
        if cur is not None and cur["c1"] == c0 and (c0 + w - cur["c0"]) <= cx.wcols and len(cur["segs"]) < 2:
            cur["segs"].append((si, c0, w))
            cur["c1"] = c0 + w
        else:
            cur = {"c0": c0, "c1": c0 + w, "segs": [(si, c0, w)]}
            groups.append(cur)
    kgroups = []
    i = 0
    while i < len(kch):
        if kch[i][1] == 128:
            j = i
            while j < len(kch) and j - i < cx.wk and kch[j][1] == 128 and kch[j][0] == kch[i][0] + (j - i) * 128:
                j += 1
            kgroups.append(list(range(i, j)))
            i = j
        else:
            kgroups.append([i])
            i += 1
    nk = len(kch)
    for g in groups:
        banks = [cx.pl[(cx.gi * 2 + t) % 4] for t in range(2)]
        cx.gi += 1
        gw = g["c1"] - g["c0"]
        for kg in kgroups:
            st = cx.stage[cx.wi % 2]
            wb = cx.wbf[cx.wi % 2]
            cx.wi += 1
            r0 = kch[kg[0]][0]
            rows = kch[kg[0]][1]
            n = len(kg)
            if rows == 128:
                src = W[r0:r0 + n * 128, g["c0"]:g["c1"]].rearrange("(k p) c -> p k c", p=128)
                P.dma("sync", st[:, 0:n, 0:gw], src)
                ce = "vector" if cx.cast_i % 2 == 0 else "gpsimd"
                cx.cast_i += 1
                P.copy(wb[:, 0:n, 0:gw], st[:, 0:n, 0:gw], eng=ce)
            else:
                src = W[r0:r0 + rows, g["c0"]:g["c1"]]
                P.dma("sync", st[0:rows, 0, 0:gw], src)
                P.copy(wb[0:rows, 0, 0:gw], st[0:rows, 0, 0:gw], eng="vector")
            for t, (si, c0, w) in enumerate(g["segs"]):
                off = c0 - g["c0"]
                for kk, ki in enumerate(kg):
                    rws = kch[ki][1]
                    P.mm(banks[t][0:w, 0:T], wb[0:rws, kk, off:off + w], rhs_fn(ki),
                         start=(ki == 0), stop=(ki == nk - 1))
        for t, (si, c0, w) in enumerate(g["segs"]):
            consume(si, banks[t][0:w, 0:T])


def full_k(K):
    return [(i * 128, min(128, K - i * 128)) for i in range((K + 127) // 128)]


def full_segs(c0, n):
    out = []
    c = c0
    while c < c0 + n:
        w = min(128, c0 + n - c)
        out.append((c, w))
        c += w
    return out


def rms_stats(cx, ps_stat, src_fn, nch, sq_tile, Dn, eps, rstd_out, T=None):
    P = cx.P
    T = T or cx.T
    for c in range(nch):
        sq = sq_tile[c % 2]
        P.act(sq[:, 0:T], src_fn(c), AF.Square)
        P.mm(ps_stat[:, 0:T], cx.ones[:, :], sq[:, 0:T], start=(c == 0), stop=(c == nch - 1))
    P.ts(rstd_out, ps_stat[:, 0:T], 1.0 / Dn, eps, op0=ALU.mult, op1=ALU.add)
    P.act(rstd_out, rstd_out, AF.Sqrt)
    P.op("vector", "reciprocal", out=rstd_out, in_=rstd_out)


FF = 5632
NFC = FF // 128
EPS = 1e-6


def build_first(ntok):
    nc = bass.Bass("TRN2", target_bir_lowering=False)
    hT = nc.dram_tensor("hT", [D, ntok], F32, kind="ExternalInput").ap()
    gains = nc.dram_tensor("gains", [128, 16], F32, kind="ExternalInput").ap()
    xo = nc.dram_tensor("xnTo", [D, ntok], BF16, kind="ExternalOutput").ap()
    P = Prog(nc)
    T = TB
    ones = P.sb("ones_bf", [128, 128], BF16); P.memset(ones[:], 1.0)

    class _C:
        pass
    cx = _C(); cx.P = P; cx.T = T; cx.ones = ones
    h = [P.sb("h%d" % i, [128, NKC, T], F32) for i in range(2)]
    xb = [P.sb("xb%d" % i, [128, NKC, T], BF16) for i in range(2)]
    sq = [P.sb("sq%d" % i, [128, T], BF16) for i in range(2)]
    rstd = P.sb("rstd", [128, T], F32)
    gn = P.sb("gn", [128, 16], F32)
    pstat = P.ps("pstat", [128, 512], F32)
    P.dma("gpsimd", gn[:], gains)
    hT3 = hT.rearrange("(k p) t -> p k t", p=128)
    xo3 = xo.rearrange("(k p) t -> p k t", p=128)
    outs = []
    for b in range(ntok // T):
        ts_ = slice(b * T, (b + 1) * T)
        hh = h[b % 2]; xx = xb[b % 2]
        P.dma("sync", hh[:, 0:8, :], hT3[:, 0:8, ts_])
        P.dma("sync", hh[:, 8:16, :], hT3[:, 8:16, ts_])
        rms_stats(cx, pstat, lambda c: hh[:, c, :], NKC, sq, D, EPS, rstd[:, :])
        for c in range(NKC):
            P.stt(xx[:, c, :], hh[:, c, :], gn[:, c:c + 1], rstd[:, :], ALU.mult, ALU.mult, eng="vector")
        outs.append(P.dma("gpsimd", xo3[:, :, ts_], xx[:, :, :]))
    P.finish(outs)
    return nc, P


def build_tl(ntok):
    nc = bass.Bass("TRN2", target_bir_lowering=False)
    hT = nc.dram_tensor("hT", [D, ntok], F32, kind="ExternalInput").ap()
    moT = nc.dram_tensor("moT", [D, ntok], BF16, kind="ExternalInput").ap()
    wo = nc.dram_tensor("wo", [D, D], F32, kind="ExternalInput").ap()
    wg = nc.dram_tensor("wg", [D, FF], F32, kind="ExternalInput").ap()
    wu = nc.dram_tensor("wu", [D, FF], F32, kind="ExternalInput").ap()
    wd = nc.dram_tensor("wd", [FF, D], F32, kind="ExternalInput").ap()
    gains = nc.dram_tensor("gains", [128, 64], F32, kind="ExternalInput").ap()
    hTo = nc.dram_tensor("hTo", [D, ntok], F32, kind="ExternalOutput").ap()
    xo = nc.dram_tensor("xnTo", [D, ntok], BF16, kind="ExternalOutput").ap()
    P = Prog(nc)
    cx = Ctx(P)
    T = TB
    h = P.sb("h", [128, NKC, T], F32)
    mx = P.sb("mx", [128, NKC, T], F32)
    xb = P.sb("xb", [128, NKC, T], BF16)
    hid = P.sb("hid", [128, NFC, T], BF16)
    sq = [P.sb("sq%d" % i, [128, T], BF16) for i in range(2)]
    rstd = P.sb("rstd", [128, T], F32)
    tmp = [P.sb("tmp%d" % i, [128, T], F32) for i in range(2)]
    gn = P.sb("gn", [128, 64], F32)
    pstat = P.ps("pstat", [128, 512], F32)
    P.dma("gpsimd", gn[:], gains)
    outs = []
    hT3 = hT.rearrange("(k p) t -> p k t", p=128)
    moT3 = moT.rearrange("(k p) t -> p k t", p=128)
    hTo3 = hTo.rearrange("(k p) t -> p k t", p=128)
    xo3 = xo.rearrange("(k p) t -> p k t", p=128)

    def post_norm_residual(goff):
        rms_stats(cx, pstat, lambda c: mx[:, c, :], NKC, sq, D, EPS, rstd[:, :])
        for c in range(NKC):
            t = tmp[c % 2]
            P.stt(t[:, :], mx[:, c, :], gn[:, goff + c:goff + c + 1], rstd[:, :], ALU.mult, ALU.mult, eng="vector")
            P.tt(h[:, c, :], h[:, c, :], t[:, :], ALU.add, eng="gpsimd")

    def cons_o(si, ps):
        P.copy(mx[:, si, :], ps, eng="scalar")

    for b in range(ntok // T):
        ts_ = slice(b * T, (b + 1) * T)
        for half in range(2):
            ks = slice(half * 8, half * 8 + 8)
            P.dma("gpsimd", h[:, ks, :], hT3[:, ks, ts_])
        P.dma("gpsimd", xb[:, :, :], moT3[:, :, ts_])
        linear(cx, wo, full_k(D), full_segs(0, D), lambda k: xb[:, k, :], cons_o)
        post_norm_residual(0)
        rms_stats(cx, pstat, lambda c: h[:, c, :], NKC, sq, D, EPS, rstd[:, :])
        for c in range(NKC):
            P.stt(xb[:, c, :], h[:, c, :], gn[:, 16 + c:17 + c], rstd[:, :], ALU.mult, ALU.mult, eng="vector")
        for j0 in range(0, NFC, 2):
            gt = {}

            def cons_g(si, ps, gt=gt):
                t = tmp[si % 2]
                P.act(t[:, :], ps, AF.Silu)
                gt[si] = t

            def cons_u(si, ps, j0=j0, gt=gt):
                P.tt(hid[:, j0 + si, :], gt[si][:, :], ps, ALU.mult, eng="vector")
            linear(cx, wg, full_k(D), full_segs(j0 * 128, 256), lambda k: xb[:, k, :], cons_g)
            linear(cx, wu, full_k(D), full_segs(j0 * 128, 256), lambda k: xb[:, k, :], cons_u)
        linear(cx, wd, full_k(FF), full_segs(0, D), lambda k: hid[:, k, :], cons_o)
        post_norm_residual(32)
        for half in range(2):
            ks = slice(half * 8, half * 8 + 8)
            outs.append(P.dma("gpsimd", hTo3[:, ks, ts_], h[:, ks, :]))
        rms_stats(cx, pstat, lambda c: h[:, c, :], NKC, sq, D, EPS, rstd[:, :])
        for c in range(NKC):
            P.stt(xb[:, c, :], h[:, c, :], gn[:, 48 + c:49 + c], rstd[:, :], ALU.mult, ALU.mult, eng="vector")
        outs.append(P.dma("gpsimd", xo3[:, :, ts_], xb[:, :, :]))
    P.finish(outs)
    return nc, P


C = 64
NH = 8
CH = 512
RW_T = 256
NCK = RW_T // C
HW = NH * C
M_NL, M_NU, M_U, M_UI, M_NUI, M_ID8 = [i * HW for i in range(6)]
O_IDENT = 6 * HW
O_RESET = O_IDENT + 128
CST_W = O_RESET + RW_T
P_W0, P_A0, P_KK, P_KA, P_RK, P_LNW, P_LNB, P_V0 = range(8)


def rwkv_consts():
    c = np.zeros((128, CST_W), np.float32)
    s = np.arange(64)[:, None]
    j = np.arange(64)[None, :]
    L = (j < s).astype(np.float32)
    U = (j > s).astype(np.float32)
    UI = (j >= s).astype(np.float32)
    I = np.eye(64, dtype=np.float32)
    for k, m in ((M_NL, -L), (M_NU, -U), (M_U, U), (M_UI, UI), (M_NUI, -UI), (M_ID8, I)):
        c[0:64, k:k + HW] = np.tile(m, (1, NH))
    c[:, O_IDENT:O_IDENT + 128] = np.eye(128, dtype=np.float32)
    r = np.ones(RW_T, np.float32)
    r[::64] = 0.0
    c[:, O_RESET:O_RESET + RW_T] = r[None, :]
    return c


def build_rwkv(S, has_vres, stage=9):
    nc = bass.Bass("TRN2", target_bir_lowering=False)
    T = RW_T

    def din(name, shape, dt=F32):
        return nc.dram_tensor(name, list(shape), dt, kind="ExternalInput").ap()
    xnT = din("xnT", [D, S + 1], BF16)
    w_r = din("w_r", [D, CH]); w_k = din("w_k", [D, CH]); w_v = din("w_v", [D, CH])
    w1 = din("w1", [D, 96]); w2 = din("w2", [96, CH])
    a1 = din("a1", [D, 96]); a2 = din("a2", [96, CH])
    g1 = din("g1", [D, 256]); g2 = din("g2", [256, CH])
    if has_vres:
        v1 = din("v1", [D, 64]); v2 = din("v2", [64, CH]); vfT = din("vfT", [CH, S])
    mixd = din("mix", [128, 96])
    chpd = din("chp", [64, 64])
    cstd = din("cst", [128, CST_W])
    moT = nc.dram_tensor("moT", [CH, S], BF16, kind="ExternalOutput").ap()
    voT = None if has_vres else nc.dram_tensor("voT", [CH, S], F32, kind="ExternalOutput").ap()
    P = Prog(nc)
    cx = Ctx(P, T=T, wcols=256, wk=4)
    ps = cx.pl + [P.ps("px%d" % i, [128, 512], F32) for i in range(4)]
    cst = P.sb("cst_s", [128, CST_W]); P.dma("gpsimd", cst[:], cstd)
    mix = P.sb("mix_s", [128, 96]); P.dma("gpsimd", mix[:], mixd)
    chp = P.sb("chp_s", [64, 72]); P.dma("gpsimd", chp[:, 0:64], chpd)
    P.ts(chp[:, 64:72], chp[:, P_KA * 8:P_KA * 8 + 8], -1.0, 1.0, op0=ALU.mult, op1=ALU.add)
    ident = cst[:, O_IDENT:O_IDENT + 128]

    def par(pi, cc):
        return chp[:, pi * 8 + cc:pi * 8 + cc + 1]

    xa = P.sb("xa", [128, NKC, T + 1], BF16)
    xx = P.sb("xx", [128, NKC, T], BF16)
    xm = [P.sb("xm0", [128, NKC, T], BF16)] * 2
    f32a = {n: P.sb(n, [64, 8, T], F32) for n in ("rT", "kT", "vT", "aT", "gT", "ld", "bon", "khf", "bhf")}
    bfa = {n: P.sb(n, [64, 8, T], BF16) for n in ("At", "Rt", "Kt", "Bt")}
    tk = {n: P.sb(n, [64, NCK, CH], BF16) for n in ("Kh", "Bh", "Vt")}
    tmpf = [P.sb("tf%d" % i, [128, T], F32) for i in range(8)]
    tmpb = [P.sb("tb%d" % i, [128, T], BF16) for i in range(2)]
    h1 = P.sb("h1", [128, 2, T], BF16)
    gcol = P.sb("gcol", [64, 8, NCK], F32)
    gm = {n: [P.sb("%s%d" % (n, c), [64, HW], BF16) for c in range(NCK)] for n in ("TT", "Mak", "Mrk", "Mrb")}
    pw = {n: [P.sb("%s%d" % (n, c), [64, HW], BF16) for c in range(2)] * (NCK // 2) for n in ("Pa", "PaT", "Pb", "PbT")}
    x1 = P.sb("x1", [64, HW], BF16)
    wt = P.sb("wt", [64, HW], BF16)
    yt = P.sb("yt", [64, HW], F32)
    yn = P.sb("yn", [64, HW], F32)
    st8 = [P.sb("st8_%d" % i, [64, NH], F32) for i in range(4)]
    Z = P.sb("Z", [64, 8, C], F32)
    Zb = P.sb("Zb", [64, 8, C], BF16)
    P.memset(Z[:], 0.0)
    P.memset(Zb[:], 0.0)
    moS = P.sb("moS", [64, 8, T], BF16)
    outs = []
    xn3 = xnT.rearrange("(k p) t -> p k t", p=128)
    mo3 = moT.rearrange("(c p) t -> p c t", p=64)
    vo3 = None if has_vres else voT.rearrange("(c p) t -> p c t", p=64)
    ei = [0]

    def eng2():
        ei[0] += 1
        return "vector" if ei[0] % 2 else "gpsimd"

    def hsl(h):
        return slice(0, 64), h

    for b in range(S // T):
        t0 = b * T
        P.dma("gpsimd", xa[:, 0:8, :], xn3[:, 0:8, t0:t0 + T + 1])
        P.dma("gpsimd", xa[:, 8:16, :], xn3[:, 8:16, t0:t0 + T + 1])
        for k in range(NKC):
            P.tt(xx[:, k, :], xa[:, k, 0:T], xa[:, k, 1:T + 1], ALU.subtract, eng=eng2())

        def mixed(i):
            m = xm[i % 2]
            for k in range(NKC):
                P.stt(m[:, k, :], xx[:, k, :], mix[:, i * 16 + k:i * 16 + k + 1], xa[:, k, 1:T + 1],
                      ALU.mult, ALU.add, eng="vector")
            return m

        def to_f32(name):
            def cons(si, psu):
                P.copy(f32a[name][:, si, :], psu, eng="scalar")
            return cons
        own = [(h * 64, 64) for h in range(NH)]
        m = mixed(0)
        linear(cx, w_r, full_k(D), own, lambda k, m=m: m[:, k, :], to_f32("rT"))
        m = mixed(1)
        linear(cx, w1, full_k(D), [(0, 96)], lambda k, m=m: m[:, k, :],
               lambda si, psu: P.act(h1[0:96, 0, :], psu, AF.Tanh))
        linear(cx, w2, [(0, 96)], own, lambda k: h1[0:96, 0, :],
               lambda si, psu: P.act(f32a["ld"][:, si, :], psu, AF.Sigmoid, bias=par(P_W0, si)))
        m = mixed(2)
        linear(cx, w_k, full_k(D), own, lambda k, m=m: m[:, k, :], to_f32("kT"))
        m = mixed(3)
        linear(cx, w_v, full_k(D), own, lambda k, m=m: m[:, k, :], to_f32("vT"))
        if has_vres:
            linear(cx, v1, full_k(D), [(0, 64)], lambda k, m=m: m[:, k, :],
                   lambda si, psu: P.copy(h1[0:64, 0, :], psu, eng="scalar"))
            vmix = f32a["bon"]
            linear(cx, v2, [(0, 64)], own, lambda k: h1[0:64, 0, :],
                   lambda si, psu: P.act(vmix[:, si, :], psu, AF.Sigmoid, bias=par(P_V0, si)))
            vf = f32a["khf"]
            P.dma("gpsimd", vf[:, :, :], vfT.rearrange("(c p) t -> p c t", p=64)[:, :, t0:t0 + T])
            for cc in range(8):
                P.tt(vf[:, cc, :], vf[:, cc, :], f32a["vT"][:, cc, :], ALU.subtract, eng="vector")
                P.tt(vf[:, cc, :], vf[:, cc, :], vmix[:, cc, :], ALU.mult, eng="vector")
                P.tt(f32a["vT"][:, cc, :], f32a["vT"][:, cc, :], vf[:, cc, :], ALU.add, eng="vector")
        if not has_vres:
            outs.append(P.dma("gpsimd", vo3[:, :, t0:t0 + T], f32a["vT"][:, :, :]))
        m = mixed(4)
        linear(cx, a1, full_k(D), [(0, 96)], lambda k, m=m: m[:, k, :],
               lambda si, psu: P.copy(h1[0:96, 0, :], psu, eng="scalar"))
        linear(cx, a2, [(0, 96)], own, lambda k: h1[0:96, 0, :],
               lambda si, psu: P.act(f32a["aT"][:, si, :], psu, AF.Sigmoid, bias=par(P_A0, si)))
        m = mixed(5)
        linear(cx, g1, full_k(D), [(0, 128), (128, 128)], lambda k, m=m: m[:, k, :],
               lambda si, psu: P.act(h1[:, si, :], psu, AF.Sigmoid))
        linear(cx, g2, [(0, 128), (128, 128)], own, lambda k: h1[:, k, :], to_f32("gT"))

        rT, kT, vT, aT, ld, bon = (f32a[n] for n in ("rT", "kT", "vT", "aT", "ld", "bon"))
        for cc in range(8):
            t_ = [x[0:64, :] for x in tmpf]
            tb_ = [x[0:64, :] for x in tmpb]
            P.ts(ld[:, cc, :], ld[:, cc, :], -0.6065306597126334, None, op0=ALU.mult, eng="gpsimd")
            P.ts(t_[0][:, :], kT[:, cc, :], par(P_KK, cc), None, op0=ALU.mult, eng="vector")
            P.act(tb_[0][:, :], t_[0][:, :], AF.Square)
            P.mm(ps[4][0:64, 0:T], cx.ones[0:64, 0:64], tb_[0][:, :])
            P.ts(t_[1][:, :], ps[4][0:64, 0:T], 1e-12, None, op0=ALU.add, eng="vector")
            P.act(t_[1][:, :], t_[1][:, :], AF.Sqrt)
            P.op("vector", "reciprocal", out=t_[1][:, :], in_=t_[1][:, :])
            P.tt(t_[0][:, :], t_[0][:, :], t_[1][:, :], ALU.mult, eng="vector")
            P.ts(t_[2][:, :], aT[:, cc, :], par(P_KA, cc), chp[:, 64 + cc:65 + cc], op0=ALU.mult, op1=ALU.add,
                 eng="gpsimd")
            P.tt(t_[2][:, :], t_[2][:, :], kT[:, cc, :], ALU.mult, eng="gpsimd")
            P.tt(t_[3][:, :], t_[0][:, :], aT[:, cc, :], ALU.mult, eng="gpsimd")
            P.stt(tb_[1][:, :], rT[:, cc, :], par(P_RK, cc), t_[2][:, :], ALU.mult, ALU.mult, eng="vector")
            P.mm(ps[5][0:64, 0:T], cx.ones[0:64, 0:64], tb_[1][:, :])
            P.tt(bon[:, cc, :], ps[5][0:64, 0:T], vT[:, cc, :], ALU.mult, eng="vector")
            cum = t_[4]
            P.op("vector", "tensor_tensor_scan", out=cum[:, :], data0=cst[0:64, O_RESET:O_RESET + T],
                 data1=ld[:, cc, :], initial=0.0, op0=ALU.mult, op1=ALU.add)
            cum3 = cum[:, :].rearrange("p (c t) -> p c t", t=C)
            clast = cum3[:, :, C - 1:C]
            P.act(gcol[:, cc, :], cum[:, :].rearrange("p (c t) -> p c t", t=C)[:, :, C - 1], AF.Exp)
            P.act(t_[5][:, :], cum[:, :], AF.Exp)
            P.tt(bfa["Rt"][:, cc, :], rT[:, cc, :], t_[5][:, :], ALU.mult, eng="vector")
            P.act(t_[5][:, :], cum[:, :], AF.Exp, scale=-1.0)
            P.tt(bfa["Kt"][:, cc, :], t_[2][:, :], t_[5][:, :], ALU.mult, eng="vector")
            P.tt(bfa["Bt"][:, cc, :], t_[3][:, :], t_[5][:, :], ALU.mult, eng="gpsimd")
            P.tt(t_[6][:, :], cum[:, :], ld[:, cc, :], ALU.subtract, eng="gpsimd")
            P.act(t_[6][:, :], t_[6][:, :], AF.Exp)
            P.tt(bfa["At"][:, cc, :], t_[0][:, :], t_[6][:, :], ALU.mult, eng="vector")
            t7 = t_[7][:, :].rearrange("p (c t) -> p c t", t=C)
            P.tt(t7, clast.broadcast_to([64, NCK, C]), cum3, ALU.subtract, eng="vector")
            P.act(t_[7][:, :], t_[7][:, :], AF.Exp)
            P.tt(f32a["khf"][:, cc, :], t_[2][:, :], t_[7][:, :], ALU.mult, eng="gpsimd")
            P.stt(f32a["bhf"][:, cc, :], t_[3][:, :], -1.0, t_[7][:, :], ALU.mult, ALU.mult, eng="vector")
        for c in range(NCK if stage >= 2 else 0):
            cs = slice(c * C, (c + 1) * C)
            for n, src in (("Kh", f32a["khf"]), ("Bh", f32a["bhf"]), ("Vt", vT)):
                bank = ps[6] if n != "Bh" else ps[7]
                for cc in range(8):
                    P.tr(bank[0:64, cc * 64:(cc + 1) * 64], src[:, cc, cs], cst[0:64, O_IDENT:O_IDENT + 64])
                P.copy(tk[n][:, c, :], bank[0:64, 0:CH], eng="scalar" if n == "Vt" else "vector")
        def gram(c):
            cs = slice(c * C, (c + 1) * C)
            specs = (("Pa", "At", "Bt", M_NL), ("PaT", "Bt", "At", M_NU), ("Mak", "Kt", "At", M_U),
                     ("Mrk", "Kt", "Rt", M_UI), ("Mrb", "Bt", "Rt", M_NUI))
            for gi, (dst, lname, rname, mo) in enumerate(specs):
                bank = ps[(c * 5 + gi) % 8]
                for h in range(NH):
                    rows, cc = hsl(h)
                    P.mm(bank[0:64, h * C:(h + 1) * C], bfa[lname][rows, cc, cs], bfa[rname][rows, cc, cs])
                dtile = (pw[dst] if dst in pw else gm[dst])[c]
                P.tt(dtile[:, :], bank[0:64, 0:HW], cst[0:64, mo:mo + HW], ALU.mult, eng="vector")
            P.tt(gm["TT"][c][:, :], pw["PaT"][c][:, :], cst[0:64, M_ID8:M_ID8 + HW], ALU.add, eng=eng2())
        for c0 in range(0, NCK if stage >= 4 else 0, 2):
            gram(c0)
            gram(c0 + 1)
            cur = {c: ("Pa", "PaT") for c in (c0, c0 + 1)}
            for lvl in range(1, 6):
                for c in (c0, c0 + 1):
                    pn, pnt = cur[c]
                    nn, nnt = ("Pb", "PbT") if pn == "Pa" else ("Pa", "PaT")
                    bk = [ps[(c - c0) * 3 + i] for i in range(3)]
                    Pm, PTm = pw[pn][c], pw[pnt][c]
                    for h in range(NH):
                        hs = slice(h * C, (h + 1) * C)
                        P.mm(bk[0][0:64, hs], PTm[:, hs], Pm[:, hs])
                    if lvl < 5:
                        for h in range(NH):
                            hs = slice(h * C, (h + 1) * C)
                            P.mm(bk[1][0:64, hs], Pm[:, hs], PTm[:, hs])
                    P.copy(pw[nn][c][:, :], bk[0][0:64, 0:HW], eng="scalar")
                    if lvl < 5:
                        P.copy(pw[nnt][c][:, :], bk[1][0:64, 0:HW], eng="vector")
                    cur[c] = (nn, nnt)
                for c in (c0, c0 + 1):
                    nn, nnt = cur[c]
                    bk = [ps[(c - c0) * 3 + i] for i in range(3)]
                    for h in range(NH):
                        hs = slice(h * C, (h + 1) * C)
                        P.mm(bk[2][0:64, hs], pw[nn][c][:, hs], gm["TT"][c][:, hs])
                    P.tt(gm["TT"][c][:, :], gm["TT"][c][:, :], bk[2][0:64, 0:HW], ALU.add, eng="vector")
        for c in range(NCK if stage >= 5 else 0):
            cs = slice(c * C, (c + 1) * C)
            for h in range(NH):
                rows, cc = hsl(h)
                hs = slice(h * C, (h + 1) * C)
                P.mm(ps[0][0:64, hs], gm["Mak"][c][:, hs], tk["Vt"][:, c, hs], start=True, stop=False)
                P.mm(ps[0][0:64, hs], bfa["At"][rows, cc, cs], Zb[rows, cc, :], start=False, stop=True)
            P.copy(x1[:, :], ps[0][0:64, 0:HW], eng="scalar")
            for h in range(NH):
                hs = slice(h * C, (h + 1) * C)
                P.mm(ps[1][0:64, hs], gm["TT"][c][:, hs], x1[:, hs])
            P.copy(wt[:, :], ps[1][0:64, 0:HW], eng="vector")
            for h in range(NH):
                rows, cc = hsl(h)
                hs = slice(h * C, (h + 1) * C)
                P.mm(ps[2][0:64, hs], bfa["Rt"][rows, cc, cs], Zb[rows, cc, :], start=True, stop=False)
                P.mm(ps[2][0:64, hs], gm["Mrk"][c][:, hs], tk["Vt"][:, c, hs], start=False, stop=False)
                P.mm(ps[2][0:64, hs], gm["Mrb"][c][:, hs], wt[:, hs], start=False, stop=True)
            for h in range(NH):
                rows, cc = hsl(h)
                hs = slice(h * C, (h + 1) * C)
                P.mm(ps[3][0:64, cc * C:(cc + 1) * C], tk["Kh"][:, c, hs], tk["Vt"][:, c, hs], start=True, stop=False)
                P.mm(ps[3][0:64, cc * C:(cc + 1) * C], tk["Bh"][:, c, hs], wt[:, hs], start=False, stop=True)
            Zf = Z[:, :, :]
            P.tt(Zf, Zf, gcol[:, :, c:c + 1].broadcast_to([64, 8, C]), ALU.mult, eng="vector")
            P.tt(Zf, Zf, ps[3][0:64, 0:8 * C].rearrange("p (c v) -> p c v", v=C), ALU.add, eng="vector")
            P.copy(Zb[:, :, :], Zf, eng="scalar")
            P.copy(yt[:, :], ps[2][0:64, 0:HW], eng="scalar")
            y3 = yt[:, :].rearrange("p (h v) -> p h v", v=C)
            P.op("vector", "tensor_reduce", out=st8[0][:, :], in_=y3, axis=AX.X, op=ALU.add)
            P.ts(st8[0][:, :], st8[0][:, :], 1.0 / C, None, op0=ALU.mult, eng="vector")
            yn3 = yn[:, :].rearrange("p (h v) -> p h v", v=C)
            P.tt(yn3, y3, st8[0][:, :].unsqueeze(2).broadcast_to([64, NH, C]), ALU.subtract, eng="vector")
            P.tt(yt[:, :], yn[:, :], yn[:, :], ALU.mult, eng="gpsimd")
            P.op("vector", "tensor_reduce", out=st8[1][:, :], in_=y3, axis=AX.X, op=ALU.add)
            P.ts(st8[1][:, :], st8[1][:, :], 1.0 / C, 64e-5, op0=ALU.mult, op1=ALU.add, eng="vector")
            P.act(st8[1][:, :], st8[1][:, :], AF.Sqrt)
            P.op("vector", "reciprocal", out=st8[1][:, :], in_=st8[1][:, :])
            P.tt(yn3, yn3, st8[1][:, :].unsqueeze(2).broadcast_to([64, NH, C]), ALU.mult, eng="vector")
            for cc in range(8):
                P.tr(ps[4][0:64, cc * C:(cc + 1) * C], yn[:, cc * 64:(cc + 1) * 64], cst[0:64, O_IDENT:O_IDENT + 64])
            for cc in range(8):
                o_ = tmpf[cc][0:64, 0:C]
                P.ts(o_, ps[4][0:64, cc * C:(cc + 1) * C], par(P_LNW, cc), par(P_LNB, cc), op0=ALU.mult, op1=ALU.add,
                     eng="vector")
                P.tt(o_, o_, f32a["bon"][:, cc, cs], ALU.add, eng="gpsimd")
                P.tt(moS[:, cc, cs], o_, f32a["gT"][:, cc, cs], ALU.mult, eng="gpsimd")
        if stage < 5:
            P.memset(moS[:, :, :], 0.0)
        outs.append(P.dma("gpsimd", mo3[:, :, t0:t0 + T], moS[:, :, :]))
    P.finish(outs)
    return nc, P


MT = 256
MC_ID = 0
MC_TRI = 128
MC_PSW = 256
MC_INVF = 320
MC_SGN = 321
MC_W = 322
ROPE_THETA = 10000.0
EPS_ = 1e-6


def mla_consts():
    c = np.zeros((128, MC_W), np.float32)
    c[:, MC_ID:MC_ID + 128] = np.eye(128, dtype=np.float32)
    k = np.arange(128)[:, None]
    q = np.arange(128)[None, :]
    c[:, MC_TRI:MC_TRI + 128] = (k <= q).astype(np.float32)
    psw = np.zeros((64, 64), np.float32)
    for i in range(32):
        psw[i + 32, i] = 1.0
        psw[i, i + 32] = 1.0
    c[0:64, MC_PSW:MC_PSW + 64] = psw
    invf = (1.0 / (ROPE_THETA ** (np.arange(0, 64, 2, dtype=np.float32) / np.float32(64)))).astype(np.float32)
    c[0:64, MC_INVF] = np.concatenate([invf, invf])
    c[0:32, MC_SGN] = 1.0
    c[32:64, MC_SGN] = -1.0
    return c


def build_mla(S):
    nc = bass.Bass("TRN2", target_bir_lowering=False)
    T = MT

    def din(name, shape, dt=F32):
        return nc.dram_tensor(name, list(shape), dt, kind="ExternalInput").ap()
    xnT = din("xnT", [D, S], BF16)
    w_cq = din("w_cq", [D, 512]); w_ckv = din("w_ckv", [D, 512]); w_kr = din("w_kr", [D, 64])
    w_uq = din("w_uq", [512, 384]); w_ukv = din("w_ukv", [512, 512])
    nrm = din("nrm", [128, 8])
    posd = din("pos", [1, S], I32)
    cstd = din("mcst", [128, MC_W])
    moT = nc.dram_tensor("moT", [256, S], BF16, kind="ExternalOutput").ap()
    P = Prog(nc)
    cx = Ctx(P, T=T, wcols=256, wk=4)
    pS = [P.ps("pS%d" % i, [128, 512], F32) for i in range(2)]
    po = [P.ps("po%d" % i, [128, 512], F32) for i in range(2)]
    cst = P.sb("mcst_s", [128, MC_W]); P.dma("gpsimd", cst[:], cstd)
    nr = P.sb("nrm_s", [128, 8]); P.dma("gpsimd", nr[:], nrm)
    ident = cst[:, MC_ID:MC_ID + 128]
    trib = P.sb("trib", [128, 128], BF16)
    P.copy(trib[:, :], cst[:, MC_TRI:MC_TRI + 128])
    xa = P.sb("xa", [128, NKC, T], BF16)
    cq = P.sb("cq", [128, 4, T], F32)
    cqb = P.sb("cqb", [128, 4, T], BF16)
    sq = [P.sb("sq%d" % i, [128, T], BF16) for i in range(2)]
    rstd = P.sb("rstd", [128, T], F32)
    qnT = [P.sb("qnT%d" % h, [128, T], BF16) for h in range(2)]
    qrT = [P.sb("qrT%d" % h, [64, T], BF16) for h in range(2)]
    xr = P.sb("xr", [64, T], F32)
    rt1 = P.sb("rt1", [64, T], F32)
    rt2 = P.sb("rt2", [64, T], F32)
    posi = P.sb("posi", [64, T], I32)
    ang = P.sb("ang", [64, T], F32)
    CF = P.sb("CF", [64, T], F32)
    SF = P.sb("SF", [64, T], F32)
    vT = P.sb("vT", [128, T], F32)
    KnT = P.sb("KnT", [128, 2, S], BF16)
    KrT = P.sb("KrT", [64, S], BF16)
    NT = S // 128
    Vx = P.sb("Vx", [128, 2, NT, 130], BF16)
    P.memset(Vx[:, :, :, 128:130], 1.0)
    pt = [P.sb("pt%d" % i, [128, T], BF16) for i in range(2)]
    rec = P.sb("rec", [128, 2], F32)
    otok = [P.sb("otok%d" % i, [128, 128], F32) for i in range(2)]
    moS = P.sb("moS", [128, 2, T], BF16)
    outs = []
    xn3 = xnT.rearrange("(k p) t -> p k t", p=128)
    mo3 = moT.rearrange("(h p) t -> p h t", p=128)
    scale = 192.0 ** -0.5
    TWO_PI = 2.0 * math.pi

    def rope(dst):
        bank = cx.pl[0]
        P.mm(bank[0:64, 0:T], cst[0:64, MC_PSW:MC_PSW + 64], xr[:, :])
        P.tt(rt1[:, :], xr[:, :], CF[:, :], ALU.mult, eng="gpsimd")
        P.tt(rt2[:, :], bank[0:64, 0:T], SF[:, :], ALU.mult, eng="vector")
        P.tt(dst, rt1[:, :], rt2[:, :], ALU.add, eng="vector")

    for b in range(S // T):
        t0 = b * T
        bs = slice(t0, t0 + T)
        P.dma("gpsimd", xa[:, 0:8, :], xn3[:, 0:8, bs])
        P.dma("gpsimd", xa[:, 8:16, :], xn3[:, 8:16, bs])
        P.dma("gpsimd", posi[:, :], posd[0:1, bs].broadcast_to([64, T]))
        P.copy(ang[:, :], posi[:, :], eng="vector")
        P.ts(ang[:, :], ang[:, :], cst[0:64, MC_INVF:MC_INVF + 1], None, op0=ALU.mult)
        C1 = 6.28125
        C2 = TWO_PI - C1
        P.ts(rt1[:, :], ang[:, :], 1.0 / TWO_PI, None, op0=ALU.mult)
        P.copy(posi[:, :], rt1[:, :], eng="vector")
        P.copy(rt1[:, :], posi[:, :], eng="vector")
        P.stt(ang[:, :], rt1[:, :], -C1, ang[:, :], ALU.mult, ALU.add)
        P.stt(ang[:, :], rt1[:, :], -C2, ang[:, :], ALU.mult, ALU.add)

        def wrap(x):
            P.ts(rt2[:, :], x, math.pi, TWO_PI, op0=ALU.is_gt, op1=ALU.mult)
            P.tt(x, x, rt2[:, :], ALU.subtract)
            P.ts(rt2[:, :], x, -math.pi, TWO_PI, op0=ALU.is_lt, op1=ALU.mult)
            P.tt(x, x, rt2[:, :], ALU.add)
        wrap(ang[:, :])
        P.act(SF[:, :], ang[:, :], AF.Sin)
        P.ts(SF[:, :], SF[:, :], cst[0:64, MC_SGN:MC_SGN + 1], -1.0, op0=ALU.mult, op1=ALU.mult)
        P.ts(rt1[:, :], ang[:, :], 0.5 * math.pi, None, op0=ALU.add)
        wrap(rt1[:, :])
        P.act(CF[:, :], rt1[:, :], AF.Sin)

        def lora(wdn, goff):
            linear(cx, wdn, full_k(D), full_segs(0, 512), lambda k: xa[:, k, :],
                   lambda si, psu: P.copy(cq[:, si, :], psu, eng="scalar"))
            rms_stats(cx, po[0], lambda c: cq[:, c, :], 4, sq, 512, EPS_, rstd[:, :])
            for c in range(4):
                P.stt(cqb[:, c, :], cq[:, c, :], nr[:, goff + c:goff + c + 1], rstd[:, :], ALU.mult, ALU.mult)
        lora(w_cq, 0)
        for h in range(2):
            def cons_q(si, psu, h=h):
                if si == 0:
                    P.copy(qnT[h][:, :], psu, eng="scalar")
                else:
                    P.copy(xr[:, :], psu, eng="scalar")
                    rope(qrT[h][:, :])
            linear(cx, w_uq, full_k(512), [(h * 192, 128), (h * 192 + 128, 64)], lambda k: cqb[:, k, :], cons_q)
        def cons_kr(si, psu):
            P.copy(xr[:, :], psu, eng="scalar")
            rope(KrT[:, bs])
        linear(cx, w_kr, full_k(D), [(0, 64)], lambda k: xa[:, k, :], cons_kr)
        lora(w_ckv, 4)
        for h in range(2):
            def cons_kv(si, psu, h=h):
                if si == 0:
                    P.copy(KnT[:, h, bs], psu, eng="scalar")
                else:
                    P.copy(vT[:, :], psu, eng="scalar")
                    for j in range(2):
                        bank = cx.pl[1 + j]
                        P.tr(bank[:, 0:128], vT[:, j * 128:(j + 1) * 128], ident)
                        P.copy(Vx[:, h, 2 * b + j, 0:128], bank[:, 0:128], eng="vector")
            linear(cx, w_ukv, full_k(512), [(h * 256, 128), (h * 256 + 128, 128)], lambda k: cqb[:, k, :], cons_kv)
        for h in range(2):
            nkt = 2 * b + 2
            for kt in range(nkt):
                ks = slice(kt * 128, (kt + 1) * 128)
                bank = pS[kt % 2]
                P.mm(bank[:, 0:T], KnT[:, h, ks], qnT[h][:, :], start=True, stop=False)
                P.mm(bank[:, 0:T], KrT[:, ks], qrT[h][:, :], start=False, stop=True)
                p_ = pt[kt % 2]
                P.act(p_[:, :], bank[:, 0:T], AF.Exp, scale=scale)
                for j in range(2):
                    qt = 2 * b + j
                    if kt > qt:
                        continue
                    js = slice(j * 128, (j + 1) * 128)
                    if kt == qt:
                        P.tt(p_[:, js], p_[:, js], trib[:, :], ALU.mult, eng="vector")
                    P.mm(po[j][:, 0:129], p_[:, js], Vx[:, h, kt, 0:129], start=(kt == 0), stop=(kt == qt))
            for j in range(2):
                P.op("vector", "reciprocal", out=rec[:, j:j + 1], in_=po[j][:, 128:129])
                P.ts(otok[j][:, :], po[j][:, 0:128], rec[:, j:j + 1], None, op0=ALU.mult)
                bank = cx.pl[3]
                P.tr(bank[:, 0:128], otok[j][:, :], ident)
                P.copy(moS[:, h, j * 128:(j + 1) * 128], bank[:, 0:128], eng="scalar")
        outs.append(P.dma("gpsimd", mo3[:, :, bs], moS[:, :, :]))
    P.finish(outs)
    return nc, P


GT = 256
GC_ID = 0
GC_ML = 128
GC_MU = 192
GC_ID8 = 256
GC_RESET = 768
GC_ONES = GC_RESET + GT
GC_W = GC_ONES + 128
NEG = -30000.0


def gdn_consts():
    c = np.zeros((128, GC_W), np.float32)
    c[:, GC_ID:GC_ID + 128] = np.eye(128, dtype=np.float32)
    i = np.arange(64)[:, None]
    j = np.arange(64)[None, :]
    c[0:64, GC_ML:GC_ML + 64] = np.where(j < i, 0.0, NEG)
    c[0:64, GC_MU:GC_MU + 64] = np.where(j >= i, 0.0, NEG)
    c[0:64, GC_ID8:GC_ID8 + 512] = np.tile(np.eye(64, dtype=np.float32), (1, 8))
    r = np.ones(GT, np.float32)
    r[::64] = 0.0
    c[0, GC_RESET:GC_RESET + GT] = r
    c[0, GC_ONES:GC_ONES + 128] = 1.0
    return c


def gdn_prep(gi, xT, w_in, conv_w, a_log, dt_bias, out_norm):
    h0 = 2 * gi
    cw = np.zeros((128, 3, 2, 4), np.float32)
    for w in range(3):
        for hh in range(2):
            base = w * 1024 + (h0 + hh) * 128
            cw[:, w, hh, :] = conv_w[:, base:base + 128].T
    wba = np.stack([w_in[:, 5184 + h0], w_in[:, 5184 + h0 + 1], w_in[:, 5192 + h0], w_in[:, 5192 + h0 + 1]], axis=1)
    hp = np.array([[a_log[h0], a_log[h0 + 1], dt_bias[h0], dt_bias[h0 + 1]]], np.float32)
    return {"xnT": xT, "wq": w_in[:, 1088 + h0 * 128:1088 + h0 * 128 + 256],
            "wk": w_in[:, 2112 + h0 * 128:2112 + h0 * 128 + 256],
            "wv": w_in[:, 3136 + h0 * 128:3136 + h0 * 128 + 256],
            "wz": w_in[:, 4160 + h0 * 128:4160 + h0 * 128 + 256],
            "wba": wba, "cw": cw.reshape(128, 24), "hp": hp,
            "onorm": np.asarray(out_norm, np.float32).reshape(128, 1), "gcst": gdn_consts()}


def build_gdn(S):
    nc = bass.Bass("TRN2", target_bir_lowering=False)
    T = GT
    NCK = T // 64

    def din(name, shape, dt=F32):
        return nc.dram_tensor(name, list(shape), dt, kind="ExternalInput").ap()
    xnT = din("xnT", [D, S], BF16)
    wq = din("wq", [D, 256]); wk = din("wk", [D, 256]); wv = din("wv", [D, 256]); wz = din("wz", [D, 256])
    wba = din("wba", [D, 4])
    cwd = din("cw", [128, 24]); hpd = din("hp", [1, 4]); ond = din("onorm", [128, 1])
    cstd = din("gcst", [128, GC_W])
    moT = nc.dram_tensor("moT", [256, S], BF16, kind="ExternalOutput").ap()
    P = Prog(nc)
    cx = Ctx(P, T=T, wcols=256, wk=4)
    pX, pE, pO, pZ = [P.ps("pg%d" % i, [128, 512], F32) for i in range(4)]
    pl = cx.pl
    cst = P.sb("gcst_s", [128, GC_W]); P.dma("gpsimd", cst[:], cstd)
    cw = P.sb("cw_s", [128, 24]); P.dma("gpsimd", cw[:], cwd)
    hp = P.sb("hp_s", [1, 4]); P.dma("gpsimd", hp[:], hpd)
    onr = P.sb("on_s", [128, 1]); P.dma("gpsimd", onr[:], ond)
    ident = cst[:, GC_ID:GC_ID + 128]
    id64 = cst[0:64, GC_ID:GC_ID + 64]
    id64b = P.sb("id64b", [64, 64], BF16); P.copy(id64b[:, :], id64)
    ones_row = cst[0:1, GC_ONES:GC_ONES + 128]
    one11 = cst[0:1, GC_ONES:GC_ONES + 1]
    reset = cst[0:1, GC_RESET:GC_RESET + T]
    nA = P.sb("nA", [1, 2], F32)
    P.act(nA[:, :], hp[0:1, 0:2], AF.Exp)
    P.ts(nA[:, :], nA[:, :], -1.0, None, op0=ALU.mult)

    xa = P.sb("xa", [128, NKC, T], BF16)
    raw = P.sb("raw", [128, 3, 2, T + 3], F32)
    P.memset(raw[:], 0.0)
    szT = P.sb("szT", [128, 2, T], F32)
    rows = {n: P.sb("r_" + n, [1, 2, T], F32) for n in ("b", "g", "gam", "eg", "ebg", "ed", "t")}
    bc = {n: P.sb("bc_" + n, [128, 2, T], F32) for n in ("eg", "ebg", "ed", "b", "gam")}
    cols = P.sb("cols", [64, 16], F32)
    ncols = P.sb("ncols", [64, 16], F32)
    cv = P.sb("cv", [128, 3, 2, T], F32)
    acc = [P.sb("acc%d" % i, [128, T], F32) for i in range(2)]
    sqb = [P.sb("sqb%d" % i, [128, T], BF16) for i in range(2)]
    rs_ = P.sb("rs_", [128, T], F32)
    qn = P.sb("qn", [128, 2, T], F32); kn = P.sb("kn", [128, 2, T], F32)
    qnb = P.sb("qnb", [128, 2, T], BF16); knb = P.sb("knb", [128, 2, T], BF16)
    qd = P.sb("qd", [128, 2, T], BF16); nkbg = P.sb("nkbg", [128, 2, T], BF16)
    kdF = P.sb("kdF", [128, 2, T], F32); vbF = P.sb("vbF", [128, 2, T], F32)
    kdt = P.sb("kdt", [64, 8, 128], BF16); vbt = P.sb("vbt", [64, 8, 128], BF16)
    dx = P.sb("dx", [64, 4, 64], F32)
    Dl = P.sb("Dl", [64, 8, 64], F32); Du = P.sb("Du", [64, 8, 64], F32)
    P0f = P.sb("P0f", [64, 8, 64], F32)
    attnT = P.sb("attnT", [64, 512], BF16)
    pwt = {n: P.sb("g" + n, [64, 512], BF16) for n in ("Pa", "PaT", "Pb", "PbT", "TT")}
    Sst = P.sb("Sst", [128, 2, 128], F32); Sb = P.sb("Sb", [128, 2, 128], BF16)
    P.memset(Sst[:], 0.0); P.memset(Sb[:], 0.0)
    x1 = P.sb("x1", [64, 256], BF16); et = P.sb("et", [64, 256], BF16)
    ot = P.sb("ot", [64, 256], F32); osq = P.sb("osq", [64, 256], F32); on = P.sb("on", [64, 256], F32)
    st2 = P.sb("st2", [64, 2], F32)
    moS = P.sb("moS", [128, 2, T], BF16)
    outs = []
    xn3 = xnT.rearrange("(k p) t -> p k t", p=128)
    mo3 = moT.rearrange("(h p) t -> p h t", p=128)

    def cwc(w, hh, j):
        i = (w * 2 + hh) * 4 + j
        return cw[:, i:i + 1]

    for b in range(S // T):
        t0 = b * T
        bs = slice(t0, t0 + T)
        P.dma("gpsimd", xa[:, 0:8, :], xn3[:, 0:8, bs])
        P.dma("gpsimd", xa[:, 8:16, :], xn3[:, 8:16, bs])
        if b > 0:
            for w in range(3):
                P.copy(raw[:, w, :, 0:3], raw[:, w, :, T:T + 3], eng="gpsimd")
        two = [(0, 128), (128, 128)]
        for w, W in enumerate((wq, wk, wv)):
            linear(cx, W, full_k(D), two, lambda k: xa[:, k, :],
                   lambda si, psu, w=w: P.copy(raw[:, w, si, 3:T + 3], psu, eng="scalar"))
        linear(cx, wz, full_k(D), two, lambda k: xa[:, k, :],
               lambda si, psu: P.act(szT[:, si, :], psu, AF.Silu))

        def cons_ba(si, psu):
            if si < 2:
                P.act(rows["b"][0:1, si, :], psu, AF.Sigmoid)
            else:
                hh = si - 2
                t = rows["t"][0:1, hh, :]
                P.act(t, psu, AF.Exp, bias=hp[0:1, 2 + hh:3 + hh])
                P.ts(t, t, 1.0, None, op0=ALU.add)
                P.act(t, t, AF.Ln)
                P.ts(rows["g"][0:1, hh, :], t, nA[0:1, hh:hh + 1], None, op0=ALU.mult)
        linear(cx, wba, full_k(D), [(0, 1), (1, 1), (2, 1), (3, 1)], lambda k: xa[:, k, :], cons_ba)
        for hh in range(2):
            P.op("vector", "tensor_tensor_scan", out=rows["gam"][0:1, hh, :], data0=reset, data1=rows["g"][0:1, hh, :],
                 initial=0.0, op0=ALU.mult, op1=ALU.add)
        P.act(rows["eg"][:, :, :], rows["gam"][:, :, :], AF.Exp)
        P.tt(rows["ebg"][:, :, :], rows["eg"][:, :, :], rows["b"][:, :, :], ALU.mult)
        g8 = rows["gam"][:, :, :].rearrange("p h (c t) -> p (h c) t", t=64)
        P.tt(rows["ed"][:, :, :].rearrange("p h (c t) -> p (h c) t", t=64), g8[:, :, 63:64].broadcast_to([1, 8, 64]), g8,
             ALU.subtract)
        P.act(rows["ed"][:, :, :], rows["ed"][:, :, :], AF.Exp)
        for i, n in enumerate(("eg", "ebg", "ed", "b", "gam")):
            bank = pl[i % 4]
            P.mm(bank[:, 0:2 * T], ones_row, rows[n][0:1, :, :].rearrange("p h t -> p (h t)"))
            P.copy(bc[n][:, :, :].rearrange("p h t -> p (h t)"), bank[:, 0:2 * T], eng="scalar" if i % 2 else "vector")
        for hh in range(2):
            for c in range(NCK):
                s = hh * 4 + c
                P.mm(pX[0:64, s:s + 1], rows["gam"][0:1, hh, c * 64:(c + 1) * 64], one11)
                P.mm(pX[0:64, 8 + s:9 + s], rows["b"][0:1, hh, c * 64:(c + 1) * 64], one11)
        P.copy(cols[:, :], pX[0:64, 0:16], eng="vector")
        P.ts(ncols[:, :], cols[:, :], -1.0, None, op0=ALU.mult)
        for w in range(3):
            for hh in range(2):
                a_ = acc[(w * 2 + hh) % 2]
                P.ts(a_[:, :], raw[:, w, hh, 0:T], cwc(w, hh, 0), None, op0=ALU.mult)
                for j in range(1, 4):
                    P.stt(a_[:, :], raw[:, w, hh, j:j + T], cwc(w, hh, j), a_[:, :], ALU.mult, ALU.add)
                P.act(cv[:, w, hh, :], a_[:, :], AF.Silu)
        for w, dst, dstb, scl in ((0, qn, qnb, 128.0 ** -0.5), (1, kn, knb, 1.0)):
            for hh in range(2):
                s_ = sqb[hh]
                P.act(s_[:, :], cv[:, w, hh, :], AF.Square)
                bank = pl[(w * 2 + hh) % 4]
                P.mm(bank[:, 0:T], cx.ones[:, :], s_[:, :])
                P.ts(rs_[:, :], bank[:, 0:T], 1e-6, None, op0=ALU.add)
                P.act(rs_[:, :], rs_[:, :], AF.Sqrt)
                P.op("vector", "reciprocal", out=rs_[:, :], in_=rs_[:, :])
                P.stt(dst[:, hh, :], cv[:, w, hh, :], scl, rs_[:, :], ALU.mult, ALU.mult)
                P.copy(dstb[:, hh, :], dst[:, hh, :], eng="gpsimd")
        for hh in range(2):
            P.tt(qd[:, hh, :], qn[:, hh, :], bc["eg"][:, hh, :], ALU.mult, eng="gpsimd")
            P.stt(nkbg[:, hh, :], kn[:, hh, :], -1.0, bc["ebg"][:, hh, :], ALU.mult, ALU.mult)
            P.tt(kdF[:, hh, :], kn[:, hh, :], bc["ed"][:, hh, :], ALU.mult, eng="gpsimd")
            P.tt(vbF[:, hh, :], cv[:, 2, hh, :], bc["b"][:, hh, :], ALU.mult, eng="gpsimd")
        for srcF, dstT, bank in ((kdF, kdt, pl[0]), (vbF, vbt, pl[1])):
            for hh in range(2):
                for c in range(NCK):
                    P.tr(bank[0:64, c * 128:(c + 1) * 128], srcF[:, hh, c * 64:(c + 1) * 64], ident)
                P.copy(dstT[:, hh * 4:(hh + 1) * 4, :].rearrange("p s v -> p (s v)"), bank[0:64, 0:512],
                       eng="scalar" if hh else "vector")
        for hh in range(2):
            sl = slice(hh * 4, hh * 4 + 4)
            gb3 = bc["gam"][0:64, hh, :].rearrange("p (c t) -> p c t", t=64)
            gc = cols[:, sl].unsqueeze(2).broadcast_to([64, 4, 64])
            P.tt(dx[:, :, :], gc, gb3, ALU.subtract)
            P.tt(dx[:, :, :], dx[:, :, :], cst[0:64, GC_ML:GC_ML + 64].unsqueeze(1).broadcast_to([64, 4, 64]), ALU.add)
            P.act(Dl[:, sl, :], dx[:, :, :], AF.Exp)
            P.tt(dx[:, :, :], gb3, gc, ALU.subtract)
            P.tt(dx[:, :, :], dx[:, :, :], cst[0:64, GC_MU:GC_MU + 64].unsqueeze(1).broadcast_to([64, 4, 64]), ALU.add)
            P.act(Du[:, sl, :], dx[:, :, :], AF.Exp)
        for hh in range(2):
            for c in range(NCK):
                s = hh * 4 + c
                cs = slice(c * 64, (c + 1) * 64)
                P.mm(pl[2][0:64, s * 64:(s + 1) * 64], knb[:, hh, cs], knb[:, hh, cs])
                P.mm(pl[3][0:64, s * 64:(s + 1) * 64], knb[:, hh, cs], qnb[:, hh, cs])
        P.tt(P0f[:, :, :], pl[2][0:64, 0:512].rearrange("p (s t) -> p s t", t=64), Dl[:, :, :], ALU.mult)
        P.tt(P0f[:, :, :], P0f[:, :, :], ncols[:, 8:16].unsqueeze(2).broadcast_to([64, 8, 64]), ALU.mult)
        P.tt(attnT[:, :].rearrange("p (s t) -> p s t", t=64), pl[3][0:64, 0:512].rearrange("p (s t) -> p s t", t=64),
             Du[:, :, :], ALU.mult)
        P.copy(pwt["Pa"][:, :], P0f[:, :, :].rearrange("p s t -> p (s t)"), eng="gpsimd")
        for s in range(8):
            P.tr(pl[0][0:64, s * 64:(s + 1) * 64], P0f[:, s, :], id64)
        P.copy(pwt["PaT"][:, :], pl[0][0:64, 0:512], eng="scalar")
        P.tt(pwt["TT"][:, :], pwt["PaT"][:, :], cst[0:64, GC_ID8:GC_ID8 + 512], ALU.add, eng="gpsimd")
        cur = ("Pa", "PaT")
        bk = (pX, pE, pO)
        for lvl in range(1, 6):
            pn, pnt = cur
            nn, nnt = ("Pb", "PbT") if pn == "Pa" else ("Pa", "PaT")
            for s in range(8):
                hs = slice(s * 64, (s + 1) * 64)
                P.mm(bk[0][0:64, hs], pwt[pnt][:, hs], pwt[pn][:, hs])
            if lvl < 5:
                for s in range(8):
                    hs = slice(s * 64, (s + 1) * 64)
                    P.mm(bk[1][0:64, hs], pwt[pn][:, hs], pwt[pnt][:, hs])
            P.copy(pwt[nn][:, :], bk[0][0:64, 0:512], eng="scalar")
            if lvl < 5:
                P.copy(pwt[nnt][:, :], bk[1][0:64, 0:512], eng="vector")
            for s in range(8):
                hs = slice(s * 64, (s + 1) * 64)
                P.mm(bk[2][0:64, hs], pwt[nn][:, hs], pwt["TT"][:, hs])
            P.tt(pwt["TT"][:, :], pwt["TT"][:, :], bk[2][0:64, 0:512], ALU.add, eng="vector")
            cur = (nn, nnt)
        for c in range(NCK):
            cs = slice(c * 64, (c + 1) * 64)
            for hh in range(2):
                s = hh * 4 + c
                vs = slice(hh * 128, (hh + 1) * 128)
                P.mm(pX[0:64, vs], nkbg[:, hh, cs], Sb[:, hh, :], start=True, stop=False)
                P.mm(pX[0:64, vs], id64b[:, :], vbt[:, s, :], start=False, stop=True)
            P.copy(x1[:, :], pX[0:64, 0:256], eng="scalar")
            for hh in range(2):
                s = hh * 4 + c
                vs = slice(hh * 128, (hh + 1) * 128)
                P.mm(pE[0:64, vs], pwt["TT"][:, s * 64:(s + 1) * 64], x1[:, vs])
            P.copy(et[:, :], pE[0:64, 0:256], eng="vector")
            for hh in range(2):
                s = hh * 4 + c
                vs = slice(hh * 128, (hh + 1) * 128)
                P.mm(pO[0:64, vs], qd[:, hh, cs], Sb[:, hh, :], start=True, stop=False)
                P.mm(pO[0:64, vs], attnT[:, s * 64:(s + 1) * 64], et[:, vs], start=False, stop=True)
                P.mm(pZ[:, vs], kdt[:, s, :], et[:, vs])
            for hh in range(2):
                vs = slice(hh * 128, (hh + 1) * 128)
                last = bc["eg"][:, hh, c * 64 + 63:c * 64 + 64]
                P.stt(Sst[:, hh, :], Sst[:, hh, :], last, pZ[:, vs], ALU.mult, ALU.add)
            P.copy(Sb[:, :, :], Sst[:, :, :], eng="scalar")
            P.copy(ot[:, :], pO[0:64, 0:256], eng="scalar")
            P.tt(osq[:, :], ot[:, :], ot[:, :], ALU.mult, eng="gpsimd")
            P.op("vector", "tensor_reduce", out=st2[:, :], in_=osq[:, :].rearrange("p (h v) -> p h v", v=128), axis=AX.X,
                 op=ALU.add)
            P.ts(st2[:, :], st2[:, :], 1.0 / 128.0, 1e-6, op0=ALU.mult, op1=ALU.add)
            P.act(st2[:, :], st2[:, :], AF.Sqrt)
            P.op("vector", "reciprocal", out=st2[:, :], in_=st2[:, :])
            P.tt(on[:, :].rearrange("p (h v) -> p h v", v=128), ot[:, :].rearrange("p (h v) -> p h v", v=128),
                 st2[:, :].unsqueeze(2).broadcast_to([64, 2, 128]), ALU.mult)
            for hh in range(2):
                P.tr(pl[1][:, hh * 64:(hh + 1) * 64], on[:, hh * 128:(hh + 1) * 128], id64)
            for hh in range(2):
                P.stt(moS[:, hh, cs], pl[1][:, hh * 64:(hh + 1) * 64], onr[:, 0:1], szT[:, hh, cs], ALU.mult, ALU.mult)
        outs.append(P.dma("gpsimd", mo3[:, :, bs], moS[:, :, :]))
    P.finish(outs)
    return nc, P


def _run(nc, maps):
    maps = [{k: np.ascontiguousarray(v) for k, v in m.items()} for m in maps]
    return run_bass_kernel_spmd(nc, maps, core_ids=list(range(len(maps)))).results


def _gl(gg):
    gg = np.asarray(gg, np.float32)
    return np.ascontiguousarray(gg.reshape(-1, 16, 128).transpose(2, 0, 1).reshape(128, -1))


def kernel(**inp):
    A = lambda n: np.asarray(inp[n])
    x = np.asarray(inp["x"], np.float32)
    B, S, _ = x.shape
    NT = B * S
    NG = 4
    hT = np.ascontiguousarray(x.reshape(NT, D).T)
    nc, _p = build_first(NT)
    xnT = _run(nc, [{"hT": hT, "gains": _gl(A("norm_mix_pre")[0:1])}])[0]["xnTo"]
    positions = np.asarray(inp["positions"]).astype(np.int32)
    vfirst = {}
    for l in range(4):
        moT = np.zeros((D, NT), xnT.dtype)
        cores = [(b, g) for b in range(B) for g in range(NG)]
        if l % 2 == 0:
            e = l // 2
            w_in = A("hyb_w_in")[e]
            uq = A("mla_w_uq")[e]
            ukv = A("mla_w_ukv")[e]
            nrm = np.concatenate([A("mla_q_norm")[e].reshape(4, 128).T, A("mla_kv_norm")[e].reshape(4, 128).T], axis=1)
            mcst = mla_consts()
            maps = []
            for b, g in cores:
                maps.append({"xnT": xnT[:, b * S:(b + 1) * S], "w_cq": w_in[:, 0:512], "w_ckv": w_in[:, 512:1024],
                             "w_kr": w_in[:, 1024:1088], "w_uq": uq[:, g * 384:(g + 1) * 384],
                             "w_ukv": ukv[:, g * 512:(g + 1) * 512], "nrm": nrm.astype(np.float32),
                             "pos": positions[b:b + 1, :], "mcst": mcst})
            nc, _p = build_mla(S)
            res = _run(nc, maps)
            for i, (b, g) in enumerate(cores):
                moT[g * 256:(g + 1) * 256, b * S:(b + 1) * S] = res[i]["moT"]
            maps = []
            for b, g in cores:
                maps.append(gdn_prep(g, xnT[:, b * S:(b + 1) * S], w_in, A("gdn_conv_w")[e], A("gdn_a_log")[e],
                                     A("gdn_dt_bias")[e], A("gdn_out_norm")[e]))
            nc, _p = build_gdn(S)
            res = _run(nc, maps)
            for i, (b, g) in enumerate(cores):
                moT[1024 + g * 256:1024 + (g + 1) * 256, b * S:(b + 1) * S] = res[i]["moT"]
            w_o = A("hyb_w_out")[e]
        else:
            o = l // 2
            vres = o > 0
            R_ = lambda n: A(n)[o]
            mix = np.ascontiguousarray(R_("rwkv_mix").reshape(6, 16, 128).transpose(2, 0, 1).reshape(128, 96))
            cstv = rwkv_consts()
            maps = []
            for b, g in cores:
                cs = slice(g * CH, (g + 1) * CH)
                pl_ = lambda v: np.asarray(v, np.float32).reshape(-1)[cs].reshape(8, 64).T
                chp = np.concatenate([pl_(R_("rwkv_w0")), pl_(R_("rwkv_a0")), pl_(R_("rwkv_k_k")), pl_(R_("rwkv_k_a")),
                                      pl_(R_("rwkv_r_k")), pl_(R_("rwkv_ln_w")), pl_(R_("rwkv_ln_b")),
                                      pl_(A("rwkv_v0")[o - 1]) if vres else np.zeros((64, 8), np.float32)],
                                     axis=1).astype(np.float32)
                xT = np.zeros((D, S + 1), xnT.dtype)
                xT[:, 1:] = xnT[:, b * S:(b + 1) * S]
                m = {"xnT": xT, "w_r": R_("rwkv_w_r")[:, cs], "w_k": R_("rwkv_w_k")[:, cs], "w_v": R_("rwkv_w_v")[:, cs],
                     "w1": R_("rwkv_w1"), "w2": R_("rwkv_w2")[:, cs], "a1": R_("rwkv_a1"), "a2": R_("rwkv_a2")[:, cs],
                     "g1": R_("rwkv_g1"), "g2": R_("rwkv_g2")[:, cs], "mix": mix, "chp": chp, "cst": cstv}
                if vres:
                    m.update({"v1": A("rwkv_v1")[o - 1], "v2": A("rwkv_v2")[o - 1][:, cs], "vfT": vfirst[(b, g)]})
                maps.append(m)
            nc, _p = build_rwkv(S, vres)
            res = _run(nc, maps)
            for i, (b, g) in enumerate(cores):
                moT[g * CH:(g + 1) * CH, b * S:(b + 1) * S] = res[i]["moT"]
                if not vres:
                    vfirst[(b, g)] = res[i]["voT"]
            w_o = A("rwkv_w_o")[o]
        gains = _gl(np.stack([A("norm_mix_post")[l], A("norm_ffn_pre")[l], A("norm_ffn_post")[l],
                              A("norm_mix_pre")[min(l + 1, 3)]]))
        nc, _p = build_tl(NT)
        r = _run(nc, [{"hT": hT, "moT": moT, "wo": w_o, "wg": A("ffn_w_gate")[l], "wu": A("ffn_w_up")[l],
                       "wd": A("ffn_w_down")[l], "gains": gains}])[0]
        hT = r["hTo"]
        xnT = r["xnTo"]
    return np.ascontiguousarray(np.asarray(hT, np.float32).T).reshape(B, S, D)
```

```python
import contextlib
import math
import numpy as np
import concourse.bass as bass
import concourse.mybir as mybir
from concourse.bass_utils import run_bass_kernel_spmd


F32 = mybir.dt.float32
BF16 = mybir.dt.bfloat16
I32 = mybir.dt.int32
AF = mybir.ActivationFunctionType
ALU = mybir.AluOpType
AX = mybir.AxisListType

_WRITE_KEYS = ("out", "accum_out", "ap")


class _Ins:
    __slots__ = ("eng", "fn", "deps", "needed", "val", "sem", "is_dma", "idx")

    def __init__(self, eng, fn, is_dma=False):
        self.eng = eng
        self.fn = fn
        self.deps = []
        self.needed = False
        self.val = None
        self.sem = None
        self.is_dma = is_dma


class _Trk:
    __slots__ = ("last_w", "reads")

    def __init__(self):
        self.last_w = None
        self.reads = []


class Prog:
    ENGS = ("tensor", "vector", "scalar", "gpsimd", "sync")

    def __init__(self, nc):
        self.nc = nc
        self.streams = {e: [] for e in self.ENGS}
        self.trk = {}
        self.stack = contextlib.ExitStack()
        self.dma_sems = {}
        self.eng_sems = {}
        self.n_ins = 0

    def sb(self, name, shape, dt=F32):
        return self.stack.enter_context(self.nc.sbuf_tensor(name, list(shape), dt))

    def ps(self, name, shape, dt=F32):
        return self.stack.enter_context(self.nc.psum_tensor(name, list(shape), dt))

    def dram(self, name, shape, dt=F32, kind="Internal"):
        return self.nc.dram_tensor(name, list(shape), dt, kind=kind)

    def _t(self, ap):
        n = ap.tensor.name
        t = self.trk.get(n)
        if t is None:
            t = self.trk[n] = _Trk()
        return t

    def _record(self, ins, reads, writes):
        eng = ins.eng
        deps = []
        for ap in reads:
            t = self._t(ap)
            w = t.last_w
            if w is not None:
                if not (w.eng == eng and eng == "tensor" and not w.is_dma):
                    deps.append(w)
        for ap in writes:
            t = self._t(ap)
            w = t.last_w
            if w is not None and (w.is_dma or ins.is_dma or w.eng != eng or eng != "tensor"):
                if not (w.is_dma and ins.is_dma and w.sem == ins.sem):
                    deps.append(w)
            for r in t.reads:
                if r.is_dma or ins.is_dma or r.eng != eng or eng != "tensor":
                    deps.append(r)
        for ap in reads:
            self._t(ap).reads.append(ins)
        for ap in writes:
            t = self._t(ap)
            t.last_w = ins
            t.reads = []
        seen = set()
        for d in deps:
            if d is ins or id(d) in seen:
                continue
            seen.add(id(d))
            d.needed = True
            ins.deps.append(d)
        self.streams[eng].append(ins)
        self.n_ins += 1
        return ins

    def op(self, eng, name, *args, extra_reads=(), extra_writes=(), **kw):
        reads, writes = list(extra_reads), list(extra_writes)
        for k, v in kw.items():
            if isinstance(v, bass.AP):
                (writes if k in _WRITE_KEYS else reads).append(v)
        for i, v in enumerate(args):
            if isinstance(v, bass.AP):
                (writes if i == 0 else reads).append(v)
        if name == "matmul" and kw.get("start") is False:
            pass

        if eng == "gpsimd":
            for v in list(kw.values()) + list(args):
                if isinstance(v, bass.AP) and "psum" in str(v.space).lower():
                    raise ValueError("gpsimd cannot access PSUM: %s" % name)

        def fn(e, name=name, args=args, kw=kw):
            return getattr(e, name)(*args, **kw)

        return self._record(_Ins(eng, fn), reads, writes)

    def dma(self, eng, out, in_, key=None, **kw):
        if key is None:
            sbside = out if str(out.space).lower().find("dram") < 0 else in_
            key = sbside.tensor.name
        ins = _Ins(eng, lambda e, out=out, in_=in_, kw=kw: e.dma_start(out=out, in_=in_, **kw), is_dma=True)
        ins.sem = key
        ins.needed = True
        return self._record(ins, [in_], [out])

    def mm(self, out, lhsT, rhs, start=True, stop=True, **kw):
        return self.op("tensor", "matmul", out=out, lhsT=lhsT, rhs=rhs, start=start, stop=stop, **kw)

    def tr(self, out, in_, identity, **kw):
        return self.op("tensor", "transpose", out=out, in_=in_, identity=identity, **kw)

    def act(self, out, in_, func, eng="scalar", **kw):
        return self.op(eng, "activation", out=out, in_=in_, func=func, **kw)

    def tt(self, out, in0, in1, op, eng="vector"):
        return self.op(eng, "tensor_tensor", out=out, in0=in0, in1=in1, op=op)

    def ts(self, out, in0, scalar1, scalar2=None, op0=ALU.mult, op1=None, eng="vector", **kw):
        if op1 is None:
            return self.op(eng, "tensor_scalar", out=out, in0=in0, scalar1=scalar1, scalar2=scalar2, op0=op0, **kw)
        return self.op(eng, "tensor_scalar", out=out, in0=in0, scalar1=scalar1, scalar2=scalar2, op0=op0, op1=op1, **kw)

    def stt(self, out, in0, scalar, in1, op0, op1, eng="vector", **kw):
        return self.op(eng, "scalar_tensor_tensor", out=out, in0=in0, scalar=scalar, in1=in1, op0=op0, op1=op1, **kw)

    def copy(self, out, in_, eng="vector"):
        if eng == "scalar":
            return self.op("scalar", "copy", out=out, in_=in_)
        return self.op(eng, "tensor_copy", out=out, in_=in_)

    def memset(self, ap, val, eng="vector"):
        return self.op(eng, "memset", ap=ap, constant=val)

    def finish(self, final_waits=()):
        nc = self.nc
        for e in self.ENGS:
            self.eng_sems[e] = self.stack.enter_context(nc.semaphore("se_" + e))
        counts = {}
        for e in self.ENGS:
            c = 0
            for ins in self.streams[e]:
                if ins.is_dma:
                    k = ins.sem
                    if k not in self.dma_sems:
                        self.dma_sems[k] = self.stack.enter_context(nc.semaphore("sd_%d" % len(self.dma_sems)))
                    counts[k] = counts.get(k, 0) + 16
                    ins.val = counts[k]
                    ins.sem = self.dma_sems[k]
                elif ins.needed:
                    c += 1
                    ins.val = c
                    ins.sem = self.eng_sems[e]
        key_eng = {}
        for e in self.ENGS:
            for ins in self.streams[e]:
                if ins.is_dma:
                    ke = key_eng.setdefault(ins.sem.name, e)
                    assert ke == e, "DMA key shared across engines"
        self.n_waits = 0
        prog = self

        def replay(engname):
            def body(e):
                waited = {}
                for ins in prog.streams[engname]:
                    need = {}
                    for d in ins.deps:
                        nm = d.sem.name
                        if waited.get(nm, 0) < d.val and need.get(nm, (None, 0))[1] < d.val:
                            need[nm] = (d.sem, d.val)
                    for nm, (sem, val) in need.items():
                        e.wait_ge(sem, val)
                        waited[nm] = val
                        prog.n_waits += 1
                    r = ins.fn(e)
                    if ins.is_dma:
                        r.then_inc(ins.sem, 16)
                    elif ins.needed:
                        r.then_inc(ins.sem, 1)
                if engname == "sync":
                    need = {}
                    for d in final_waits:
                        nm = d.sem.name
                        if need.get(nm, (None, 0))[1] < d.val:
                            need[nm] = (d.sem, d.val)
                    for nm, (sem, val) in need.items():
                        e.wait_ge(sem, val)
            return body

        with nc.Block() as block:
            block.tensor(replay("tensor"))
            block.vector(replay("vector"))
            block.scalar(replay("scalar"))
            block.gpsimd(replay("gpsimd"))
            block.sync(replay("sync"))
        self.stack.close()


D = 2048
TB = 512
NKC = D // 128


class Ctx:
    def __init__(self, P, T=TB, wcols=256, wk=16):
        self.P = P
        self.T = T
        self.wcols = wcols
        self.wk = wk
        self.stage = [P.sb("wst%d" % i, [128, wk, wcols], F32) for i in range(2)]
        self.wbf = [P.sb("wbf%d" % i, [128, wk, wcols], BF16) for i in range(2)]
        self.pl = [P.ps("pl%d" % i, [128, 512], F32) for i in range(4)]
        self.wi = 0
        self.gi = 0
        self.cast_i = 0
        self.ones = P.sb("ones_bf", [128, 128], BF16)
        P.memset(self.ones[:], 1.0)


def linear(cx, W, kch, segs, rhs_fn, consume, T=None):
    P = cx.P
    T = T or cx.T
    groups = []
    cur = None
    for si, (c0, w) in enumerate(segs):
        if cur is not None and cur["c1"] == c0 and (c0 + w - cur["c0"]) <= cx.wcols and len(cur["segs"]) < 2:
            cur["segs"].append((si, c0, w))
            cur["c1"] = c0 + w
        else:
            cur = {"c0": c0, "c1": c0 + w, "segs": [(si, c0, w)]}
            groups.append(cur)
    kgroups = []
    i = 0
    while i < len(kch):
        if kch[i][1] == 128:
            j = i
            while j < len(kch) and j - i < cx.wk and kch[j][1] == 128 and kch[j][0] == kch[i][0] + (j - i) * 128:
                j += 1
            kgroups.append(list(range(i, j)))
            i = j
        else:
            kgroups.append([i])
            i += 1
    nk = len(kch)
    for g in groups:
        banks = [cx.pl[(cx.gi * 2 + t) % 4] for t in range(2)]
        cx.gi += 1
        gw = g["c1"] - g["c0"]
        for kg in kgroups:
            st = cx.stage[cx.wi % 2]
            wb = cx.wbf[cx.wi % 2]
            cx.wi += 1
            r0 = kch[kg[0]][0]
            rows = kch[kg[0]][1]
            n = len(kg)
            if rows == 128:
                src = W[r0:r0 + n * 128, g["c0"]:g["c1"]].rearrange("(k p) c -> p k c", p=128)
                P.dma("sync", st[:, 0:n, 0:gw], src)
                ce = "vector" if cx.cast_i % 2 == 0 else "gpsimd"
                cx.cast_i += 1
                P.copy(wb[:, 0:n, 0:gw], st[:, 0:n, 0:gw], eng=ce)
            else:
                src = W[r0:r0 + rows, g["c0"]:g["c1"]]
                P.dma("sync", st[0:rows, 0, 0:gw], src)
                P.copy(wb[0:rows, 0, 0:gw], st[0:rows, 0, 0:gw], eng="vector")
            for t, (si, c0, w) in enumerate(g["segs"]):
                off = c0 - g["c0"]
                for kk, ki in enumerate(kg):
                    rws = kch[ki][1]
                    P.mm(banks[t][0:w, 0:T], wb[0:rws, kk, off:off + w], rhs_fn(ki),
                         start=(ki == 0), stop=(ki == nk - 1))
        for t, (si, c0, w) in enumerate(g["segs"]):
            consume(si, banks[t][0:w, 0:T])


def full_k(K):
    return [(i * 128, min(128, K - i * 128)) for i in range((K + 127) // 128)]


def full_segs(c0, n):
    out = []
    c = c0
    while c < c0 + n:
        w = min(128, c0 + n - c)
        out.append((c, w))
        c += w
    return out


def rms_stats(cx, ps_stat, src_fn, nch, sq_tile, Dn, eps, rstd_out, T=None):
    P = cx.P
    T = T or cx.T
    for c in range(nch):
        sq = sq_tile[c % 2]
        P.act(sq[:, 0:T], src_fn(c), AF.Square)
        P.mm(ps_stat[:, 0:T], cx.ones[:, :], sq[:, 0:T], start=(c == 0), stop=(c == nch - 1))
    P.ts(rstd_out, ps_stat[:, 0:T], 1.0 / Dn, eps, op0=ALU.mult, op1=ALU.add)
    P.act(rstd_out, rstd_out, AF.Sqrt)
    P.op("vector", "reciprocal", out=rstd_out, in_=rstd_out)


FF = 5632
NFC = FF // 128
EPS = 1e-6


def build_first(ntok):
    nc = bass.Bass("TRN2", target_bir_lowering=False)
    hT = nc.dram_tensor("hT", [D, ntok], F32, kind="ExternalInput").ap()
    gains = nc.dram_tensor("gains", [128, 16], F32, kind="ExternalInput").ap()
    xo = nc.dram_tensor("xnTo", [D, ntok], BF16, kind="ExternalOutput").ap()
    P = Prog(nc)
    T = TB
    ones = P.sb("ones_bf", [128, 128], BF16); P.memset(ones[:], 1.0)

    class _C:
        pass
    cx = _C(); cx.P = P; cx.T = T; cx.ones = ones
    h = [P.sb("h%d" % i, [128, NKC, T], F32) for i in range(2)]
    xb = [P.sb("xb%d" % i, [128, NKC, T], BF16) for i in range(2)]
    sq = [P.sb("sq%d" % i, [128, T], BF16) for i in range(2)]
    rstd = P.sb("rstd", [128, T], F32)
    gn = P.sb("gn", [128, 16], F32)
    pstat = P.ps("pstat", [128, 512], F32)
    P.dma("gpsimd", gn[:], gains)
    hT3 = hT.rearrange("(k p) t -> p k t", p=128)
    xo3 = xo.rearrange("(k p) t -> p k t", p=128)
    outs = []
    for b in range(ntok // T):
        ts_ = slice(b * T, (b + 1) * T)
        hh = h[b % 2]; xx = xb[b % 2]
        P.dma("sync", hh[:, 0:8, :], hT3[:, 0:8, ts_])
        P.dma("sync", hh[:, 8:16, :], hT3[:, 8:16, ts_])
        rms_stats(cx, pstat, lambda c: hh[:, c, :], NKC, sq, D, EPS, rstd[:, :])
        for c in range(NKC):
            P.stt(xx[:, c, :], hh[:, c, :], gn[:, c:c + 1], rstd[:, :], ALU.mult, ALU.mult, eng="vector")
        outs.append(P.dma("gpsimd", xo3[:, :, ts_], xx[:, :, :]))
    P.finish(outs)
    return nc, P


def build_tl(ntok):
    nc = bass.Bass("TRN2", target_bir_lowering=False)
    hT = nc.dram_tensor("hT", [D, ntok], F32, kind="ExternalInput").ap()
    moT = nc.dram_tensor("moT", [D, ntok], BF16, kind="ExternalInput").ap()
    wo = nc.dram_tensor("wo", [D, D], F32, kind="ExternalInput").ap()
    wg = nc.dram_tensor("wg", [D, FF], F32, kind="ExternalInput").ap()
    wu = nc.dram_tensor("wu", [D, FF], F32, kind="ExternalInput").ap()
    wd = nc.dram_tensor("wd", [FF, D], F32, kind="ExternalInput").ap()
    gains = nc.dram_tensor("gains", [128, 64], F32, kind="ExternalInput").ap()
    hTo = nc.dram_tensor("hTo", [D, ntok], F32, kind="ExternalOutput").ap()
    xo = nc.dram_tensor("xnTo", [D, ntok], BF16, kind="ExternalOutput").ap()
    P = Prog(nc)
    cx = Ctx(P)
    T = TB
    h = P.sb("h", [128, NKC, T], F32)
    mx = P.sb("mx", [128, NKC, T], F32)
    xb = P.sb("xb", [128, NKC, T], BF16)
    hid = P.sb("hid", [128, NFC, T], BF16)
    sq = [P.sb("sq%d" % i, [128, T], BF16) for i in range(2)]
    rstd = P.sb("rstd", [128, T], F32)
    tmp = [P.sb("tmp%d" % i, [128, T], F32) for i in range(2)]
    gn = P.sb("gn", [128, 64], F32)
    pstat = P.ps("pstat", [128, 512], F32)
    P.dma("gpsimd", gn[:], gains)
    outs = []
    hT3 = hT.rearrange("(k p) t -> p k t", p=128)
    moT3 = moT.rearrange("(k p) t -> p k t", p=128)
    hTo3 = hTo.rearrange("(k p) t -> p k t", p=128)
    xo3 = xo.rearrange("(k p) t -> p k t", p=128)

    def post_norm_residual(goff):
        rms_stats(cx, pstat, lambda c: mx[:, c, :], NKC, sq, D, EPS, rstd[:, :])
        for c in range(NKC):
            t = tmp[c % 2]
            P.stt(t[:, :], mx[:, c, :], gn[:, goff + c:goff + c + 1], rstd[:, :], ALU.mult, ALU.mult, eng="vector")
            P.tt(h[:, c, :], h[:, c, :], t[:, :], ALU.add, eng="gpsimd")

    def cons_o(si, ps):
        P.copy(mx[:, si, :], ps, eng="scalar")

    for b in range(ntok // T):
        ts_ = slice(b * T, (b + 1) * T)
        for half in range(2):
            ks = slice(half * 8, half * 8 + 8)
            P.dma("gpsimd", h[:, ks, :], hT3[:, ks, ts_])
        P.dma("gpsimd", xb[:, :, :], moT3[:, :, ts_])
        linear(cx, wo, full_k(D), full_segs(0, D), lambda k: xb[:, k, :], cons_o)
        post_norm_residual(0)
        rms_stats(cx, pstat, lambda c: h[:, c, :], NKC, sq, D, EPS, rstd[:, :])
        for c in range(NKC):
            P.stt(xb[:, c, :], h[:, c, :], gn[:, 16 + c:17 + c], rstd[:, :], ALU.mult, ALU.mult, eng="vector")
        for j0 in range(0, NFC, 2):
            gt = {}

            def cons_g(si, ps, gt=gt):
                t = tmp[si % 2]
                P.act(t[:, :], ps, AF.Silu)
                gt[si] = t

            def cons_u(si, ps, j0=j0, gt=gt):
                P.tt(hid[:, j0 + si, :], gt[si][:, :], ps, ALU.mult, eng="vector")
            linear(cx, wg, full_k(D), full_segs(j0 * 128, 256), lambda k: xb[:, k, :], cons_g)
            linear(cx, wu, full_k(D), full_segs(j0 * 128, 256), lambda k: xb[:, k, :], cons_u)
        linear(cx, wd, full_k(FF), full_segs(0, D), lambda k: hid[:, k, :], cons_o)
        post_norm_residual(32)
        for half in range(2):
            ks = slice(half * 8, half * 8 + 8)
            outs.append(P.dma("gpsimd", hTo3[:, ks, ts_], h[:, ks, :]))
        rms_stats(cx, pstat, lambda c: h[:, c, :], NKC, sq, D, EPS, rstd[:, :])
        for c in range(NKC):
            P.stt(xb[:, c, :], h[:, c, :], gn[:, 48 + c:49 + c], rstd[:, :], ALU.mult, ALU.mult, eng="vector")
        outs.append(P.dma("gpsimd", xo3[:, :, ts_], xb[:, :, :]))
    P.finish(outs)
    return nc, P


C = 64
NH = 8
CH = 512
RW_T = 256
NCK = RW_T // C
HW = NH * C
M_NL, M_NU, M_U, M_UI, M_NUI, M_ID8 = [i * HW for i in range(6)]
O_IDENT = 6 * HW
O_RESET = O_IDENT + 128
CST_W = O_RESET + RW_T
P_W0, P_A0, P_KK, P_KA, P_RK, P_LNW, P_LNB, P_V0 = range(8)


def rwkv_consts():
    c = np.zeros((128, CST_W), np.float32)
    s = np.arange(64)[:, None]
    j = np.arange(64)[None, :]
    L = (j < s).astype(np.float32)
    U = (j > s).astype(np.float32)
    UI = (j >= s).astype(np.float32)
    I = np.eye(64, dtype=np.float32)
    for k, m in ((M_NL, -L), (M_NU, -U), (M_U, U), (M_UI, UI), (M_NUI, -UI), (M_ID8, I)):
        c[0:64, k:k + HW] = np.tile(m, (1, NH))
    c[:, O_IDENT:O_IDENT + 128] = np.eye(128, dtype=np.float32)
    r = np.ones(RW_T, np.float32)
    r[::64] = 0.0
    c[:, O_RESET:O_RESET + RW_T] = r[None, :]
    return c


def build_rwkv(S, has_vres, stage=9):
    nc = bass.Bass("TRN2", target_bir_lowering=False)
    T = RW_T

    def din(name, shape, dt=F32):
        return nc.dram_tensor(name, list(shape), dt, kind="ExternalInput").ap()
    xnT = din("xnT", [D, S + 1], BF16)
    w_r = din("w_r", [D, CH]); w_k = din("w_k", [D, CH]); w_v = din("w_v", [D, CH])
    w1 = din("w1", [D, 96]); w2 = din("w2", [96, CH])
    a1 = din("a1", [D, 96]); a2 = din("a2", [96, CH])
    g1 = din("g1", [D, 256]); g2 = din("g2", [256, CH])
    if has_vres:
        v1 = din("v1", [D, 64]); v2 = din("v2", [64, CH]); vfT = din("vfT", [CH, S])
    mixd = din("mix", [128, 96])
    chpd = din("chp", [64, 64])
    cstd = din("cst", [128, CST_W])
    moT = nc.dram_tensor("moT", [CH, S], BF16, kind="ExternalOutput").ap()
    voT = None if has_vres else nc.dram_tensor("voT", [CH, S], F32, kind="ExternalOutput").ap()
    P = Prog(nc)
    cx = Ctx(P, T=T, wcols=256, wk=4)
    ps = cx.pl + [P.ps("px%d" % i, [128, 512], F32) for i in range(4)]
    cst = P.sb("cst_s", [128, CST_W]); P.dma("gpsimd", cst[:], cstd)
    mix = P.sb("mix_s", [128, 96]); P.dma("gpsimd", mix[:], mixd)
    chp = P.sb("chp_s", [64, 72]); P.dma("gpsimd", chp[:, 0:64], chpd)
    P.ts(chp[:, 64:72], chp[:, P_KA * 8:P_KA * 8 + 8], -1.0, 1.0, op0=ALU.mult, op1=ALU.add)
    ident = cst[:, O_IDENT:O_IDENT + 128]

    def par(pi, cc):
        return chp[:, pi * 8 + cc:pi * 8 + cc + 1]

    xa = P.sb("xa", [128, NKC, T + 1], BF16)
    xx = P.sb("xx", [128, NKC, T], BF16)
    xm = [P.sb("xm0", [128, NKC, T], BF16)] * 2
    f32a = {n: P.sb(n, [64, 8, T], F32) for n in ("rT", "kT", "vT", "aT", "gT", "ld", "bon", "khf", "bhf")}
    bfa = {n: P.sb(n, [64, 8, T], BF16) for n in ("At", "Rt", "Kt", "Bt")}
    tk = {n: P.sb(n, [64, NCK, CH], BF16) for n in ("Kh", "Bh", "Vt")}
    tmpf = [P.sb("tf%d" % i, [128, T], F32) for i in range(8)]
    tmpb = [P.sb("tb%d" % i, [128, T], BF16) for i in range(2)]
    h1 = P.sb("h1", [128, 2, T], BF16)
    gcol = P.sb("gcol", [64, 8, NCK], F32)
    gm = {n: [P.sb("%s%d" % (n, c), [64, HW], BF16) for c in range(NCK)] for n in ("TT", "Mak", "Mrk", "Mrb")}
    pw = {n: [P.sb("%s%d" % (n, c), [64, HW], BF16) for c in range(2)] * (NCK // 2) for n in ("Pa", "PaT", "Pb", "PbT")}
    x1 = P.sb("x1", [64, HW], BF16)
    wt = P.sb("wt", [64, HW], BF16)
    yt = P.sb("yt", [64, HW], F32)
    yn = P.sb("yn", [64, HW], F32)
    st8 = [P.sb("st8_%d" % i, [64, NH], F32) for i in range(4)]
    Z = P.sb("Z", [64, 8, C], F32)
    Zb = P.sb("Zb", [64, 8, C], BF16)
    P.memset(Z[:], 0.0)
    P.memset(Zb[:], 0.0)
    moS = P.sb("moS", [64, 8, T], BF16)
    outs = []
    xn3 = xnT.rearrange("(k p) t -> p k t", p=128)
    mo3 = moT.rearrange("(c p) t -> p c t", p=64)
    vo3 = None if has_vres else voT.rearrange("(c p) t -> p c t", p=64)
    ei = [0]

    def eng2():
        ei[0] += 1
        return "vector" if ei[0] % 2 else "gpsimd"

    def hsl(h):
        return slice(0, 64), h

    for b in range(S // T):
        t0 = b * T
        P.dma("gpsimd", xa[:, 0:8, :], xn3[:, 0:8, t0:t0 + T + 1])
        P.dma("gpsimd", xa[:, 8:16, :], xn3[:, 8:16, t0:t0 + T + 1])
        for k in range(NKC):
            P.tt(xx[:, k, :], xa[:, k, 0:T], xa[:, k, 1:T + 1], ALU.subtract, eng=eng2())

        def mixed(i):
            m = xm[i % 2]
            for k in range(NKC):
                P.stt(m[:, k, :], xx[:, k, :], mix[:, i * 16 + k:i * 16 + k + 1], xa[:, k, 1:T + 1],
                      ALU.mult, ALU.add, eng="vector")
            return m

        def to_f32(name):
            def cons(si, psu):
                P.copy(f32a[name][:, si, :], psu, eng="scalar")
            return cons
        own = [(h * 64, 64) for h in range(NH)]
        m = mixed(0)
        linear(cx, w_r, full_k(D), own, lambda k, m=m: m[:, k, :], to_f32("rT"))
        m = mixed(1)
        linear(cx, w1, full_k(D), [(0, 96)], lambda k, m=m: m[:, k, :],
               lambda si, psu: P.act(h1[0:96, 0, :], psu, AF.Tanh))
        linear(cx, w2, [(0, 96)], own, lambda k: h1[0:96, 0, :],
               lambda si, psu: P.act(f32a["ld"][:, si, :], psu, AF.Sigmoid, bias=par(P_W0, si)))
        m = mixed(2)
        linear(cx, w_k, full_k(D), own, lambda k, m=m: m[:, k, :], to_f32("kT"))
        m = mixed(3)
        linear(cx, w_v, full_k(D), own, lambda k, m=m: m[:, k, :], to_f32("vT"))
        if has_vres:
            linear(cx, v1, full_k(D), [(0, 64)], lambda k, m=m: m[:, k, :],
                   lambda si, psu: P.copy(h1[0:64, 0, :], psu, eng="scalar"))
            vmix = f32a["bon"]
            linear(cx, v2, [(0, 64)], own, lambda k: h1[0:64, 0, :],
                   lambda si, psu: P.act(vmix[:, si, :], psu, AF.Sigmoid, bias=par(P_V0, si)))
            vf = f32a["khf"]
            P.dma("gpsimd", vf[:, :, :], vfT.rearrange("(c p) t -> p c t", p=64)[:, :, t0:t0 + T])
            for cc in range(8):
                P.tt(vf[:, cc, :], vf[:, cc, :], f32a["vT"][:, cc, :], ALU.subtract, eng="vector")
                P.tt(vf[:, cc, :], vf[:, cc, :], vmix[:, cc, :], ALU.mult, eng="vector")
                P.tt(f32a["vT"][:, cc, :], f32a["vT"][:, cc, :], vf[:, cc, :], ALU.add, eng="vector")
        if not has_vres:
            outs.append(P.dma("gpsimd", vo3[:, :, t0:t0 + T], f32a["vT"][:, :, :]))
        m = mixed(4)
        linear(cx, a1, full_k(D), [(0, 96)], lambda k, m=m: m[:, k, :],
               lambda si, psu: P.copy(h1[0:96, 0, :], psu, eng="scalar"))
        linear(cx, a2, [(0, 96)], own, lambda k: h1[0:96, 0, :],
               lambda si, psu: P.act(f32a["aT"][:, si, :], psu, AF.Sigmoid, bias=par(P_A0, si)))
        m = mixed(5)
        linear(cx, g1, full_k(D), [(0, 128), (128, 128)], lambda k, m=m: m[:, k, :],
               lambda si, psu: P.act(h1[:, si, :], psu, AF.Sigmoid))
        linear(cx, g2, [(0, 128), (128, 128)], own, lambda k: h1[:, k, :], to_f32("gT"))

        rT, kT, vT, aT, ld, bon = (f32a[n] for n in ("rT", "kT", "vT", "aT", "ld", "bon"))
        for cc in range(8):
            t_ = [x[0:64, :] for x in tmpf]
            tb_ = [x[0:64, :] for x in tmpb]
            P.ts(ld[:, cc, :], ld[:, cc, :], -0.6065306597126334, None, op0=ALU.mult, eng="gpsimd")
            P.ts(t_[0][:, :], kT[:, cc, :], par(P_KK, cc), None, op0=ALU.mult, eng="vector")
            P.act(tb_[0][:, :], t_[0][:, :], AF.Square)
            P.mm(ps[4][0:64, 0:T], cx.ones[0:64, 0:64], tb_[0][:, :])
            P.ts(t_[1][:, :], ps[4][0:64, 0:T], 1e-12, None, op0=ALU.add, eng="vector")
            P.act(t_[1][:, :], t_[1][:, :], AF.Sqrt)
            P.op("vector", "reciprocal", out=t_[1][:, :], in_=t_[1][:, :])
            P.tt(t_[0][:, :], t_[0][:, :], t_[1][:, :], ALU.mult, eng="vector")
            P.ts(t_[2][:, :], aT[:, cc, :], par(P_KA, cc), chp[:, 64 + cc:65 + cc], op0=ALU.mult, op1=ALU.add,
                 eng="gpsimd")
            P.tt(t_[2][:, :], t_[2][:, :], kT[:, cc, :], ALU.mult, eng="gpsimd")
            P.tt(t_[3][:, :], t_[0][:, :], aT[:, cc, :], ALU.mult, eng="gpsimd")
            P.stt(tb_[1][:, :], rT[:, cc, :], par(P_RK, cc), t_[2][:, :], ALU.mult, ALU.mult, eng="vector")
            P.mm(ps[5][0:64, 0:T], cx.ones[0:64, 0:64], tb_[1][:, :])
            P.tt(bon[:, cc, :], ps[5][0:64, 0:T], vT[:, cc, :], ALU.mult, eng="vector")
            cum = t_[4]
            P.op("vector", "tensor_tensor_scan", out=cum[:, :], data0=cst[0:64, O_RESET:O_RESET + T],
                 data1=ld[:, cc, :], initial=0.0, op0=ALU.mult, op1=ALU.add)
            cum3 = cum[:, :].rearrange("p (c t) -> p c t", t=C)
            clast = cum3[:, :, C - 1:C]
            P.act(gcol[:, cc, :], cum[:, :].rearrange("p (c t) -> p c t", t=C)[:, :, C - 1], AF.Exp)
            P.act(t_[5][:, :], cum[:, :], AF.Exp)
            P.tt(bfa["Rt"][:, cc, :], rT[:, cc, :], t_[5][:, :], ALU.mult, eng="vector")
            P.act(t_[5][:, :], cum[:, :], AF.Exp, scale=-1.0)
            P.tt(bfa["Kt"][:, cc, :], t_[2][:, :], t_[5][:, :], ALU.mult, eng="vector")
            P.tt(bfa["Bt"][:, cc, :], t_[3][:, :], t_[5][:, :], ALU.mult, eng="gpsimd")
            P.tt(t_[6][:, :], cum[:, :], ld[:, cc, :], ALU.subtract, eng="gpsimd")
            P.act(t_[6][:, :], t_[6][:, :], AF.Exp)
            P.tt(bfa["At"][:, cc, :], t_[0][:, :], t_[6][:, :], ALU.mult, eng="vector")
            t7 = t_[7][:, :].rearrange("p (c t) -> p c t", t=C)
            P.tt(t7, clast.broadcast_to([64, NCK, C]), cum3, ALU.subtract, eng="vector")
            P.act(t_[7][:, :], t_[7][:, :], AF.Exp)
            P.tt(f32a["khf"][:, cc, :], t_[2][:, :], t_[7][:, :], ALU.mult, eng="gpsimd")
            P.stt(f32a["bhf"][:, cc, :], t_[3][:, :], -1.0, t_[7][:, :], ALU.mult, ALU.mult, eng="vector")
        for c in range(NCK if stage >= 2 else 0):
            cs = slice(c * C, (c + 1) * C)
            for n, src in (("Kh", f32a["khf"]), ("Bh", f32a["bhf"]), ("Vt", vT)):
                bank = ps[6] if n != "Bh" else ps[7]
                for cc in range(8):
                    P.tr(bank[0:64, cc * 64:(cc + 1) * 64], src[:, cc, cs], cst[0:64, O_IDENT:O_IDENT + 64])
                P.copy(tk[n][:, c, :], bank[0:64, 0:CH], eng="scalar" if n == "Vt" else "vector")
        def gram(c):
            cs = slice(c * C, (c + 1) * C)
            specs = (("Pa", "At", "Bt", M_NL), ("PaT", "Bt", "At", M_NU), ("Mak", "Kt", "At", M_U),
                     ("Mrk", "Kt", "Rt", M_UI), ("Mrb", "Bt", "Rt", M_NUI))
            for gi, (dst, lname, rname, mo) in enumerate(specs):
                bank = ps[(c * 5 + gi) % 8]
                for h in range(NH):
                    rows, cc = hsl(h)
                    P.mm(bank[0:64, h * C:(h + 1) * C], bfa[lname][rows, cc, cs], bfa[rname][rows, cc, cs])
                dtile = (pw[dst] if dst in pw else gm[dst])[c]
                P.tt(dtile[:, :], bank[0:64, 0:HW], cst[0:64, mo:mo + HW], ALU.mult, eng="vector")
            P.tt(gm["TT"][c][:, :], pw["PaT"][c][:, :], cst[0:64, M_ID8:M_ID8 + HW], ALU.add, eng=eng2())
        for c0 in range(0, NCK if stage >= 4 else 0, 2):
            gram(c0)
            gram(c0 + 1)
            cur = {c: ("Pa", "PaT") for c in (c0, c0 + 1)}
            for lvl in range(1, 6):
                for c in (c0, c0 + 1):
                    pn, pnt = cur[c]
                    nn, nnt = ("Pb", "PbT") if pn == "Pa" else ("Pa", "PaT")
                    bk = [ps[(c - c0) * 3 + i] for i in range(3)]
                    Pm, PTm = pw[pn][c], pw[pnt][c]
                    for h in range(NH):
                        hs = slice(h * C, (h + 1) * C)
                        P.mm(bk[0][0:64, hs], PTm[:, hs], Pm[:, hs])
                    if lvl < 5:
                        for h in range(NH):
                            hs = slice(h * C, (h + 1) * C)
                            P.mm(bk[1][0:64, hs], Pm[:, hs], PTm[:, hs])
                    P.copy(pw[nn][c][:, :], bk[0][0:64, 0:HW], eng="scalar")
                    if lvl < 5:
                        P.copy(pw[nnt][c][:, :], bk[1][0:64, 0:HW], eng="vector")
                    cur[c] = (nn, nnt)
                for c in (c0, c0 + 1):
                    nn, nnt = cur[c]
                    bk = [ps[(c - c0) * 3 + i] for i in range(3)]
                    for h in range(NH):
                        hs = slice(h * C, (h + 1) * C)
                        P.mm(bk[2][0:64, hs], pw[nn][c][:, hs], gm["TT"][c][:, hs])
                    P.tt(gm["TT"][c][:, :], gm["TT"][c][:, :], bk[2][0:64, 0:HW], ALU.add, eng="vector")
        for c in range(NCK if stage >= 5 else 0):
            cs = slice(c * C, (c + 1) * C)
            for h in range(NH):
                rows, cc = hsl(h)
                hs = slice(h * C, (h + 1) * C)
                P.mm(ps[0][0:64, hs], gm["Mak"][c][:, hs], tk["Vt"][:, c, hs], start=True, stop=False)
                P.mm(ps[0][0:64, hs], bfa["At"][rows, cc, cs], Zb[rows, cc, :], start=False, stop=True)
            P.copy(x1[:, :], ps[0][0:64, 0:HW], eng="scalar")
            for h in range(NH):
                hs = slice(h * C, (h + 1) * C)
                P.mm(ps[1][0:64, hs], gm["TT"][c][:, hs], x1[:, hs])
            P.copy(wt[:, :], ps[1][0:64, 0:HW], eng="vector")
            for h in range(NH):
                rows, cc = hsl(h)
                hs = slice(h * C, (h + 1) * C)
                P.mm(ps[2][0:64, hs], bfa["Rt"][rows, cc, cs], Zb[rows, cc, :], start=True, stop=False)
                P.mm(ps[2][0:64, hs], gm["Mrk"][c][:, hs], tk["Vt"][:, c, hs], start=False, stop=False)
                P.mm(ps[2][0:64, hs], gm["Mrb"][c][:, hs], wt[:, hs], start=False, stop=True)
            for h in range(NH):
                rows, cc = hsl(h)
                hs = slice(h * C, (h + 1) * C)
                P.mm(ps[3][0:64, cc * C:(cc + 1) * C], tk["Kh"][:, c, hs], tk["Vt"][:, c, hs], start=True, stop=False)
                P.mm(ps[3][0:64, cc * C:(cc + 1) * C], tk["Bh"][:, c, hs], wt[:, hs], start=False, stop=True)
            Zf = Z[:, :, :]
            P.tt(Zf, Zf, gcol[:, :, c:c + 1].broadcast_to([64, 8, C]), ALU.mult, eng="vector")
            P.tt(Zf, Zf, ps[3][0:64, 0:8 * C].rearrange("p (c v) -> p c v", v=C), ALU.add, eng="vector")
            P.copy(Zb[:, :, :], Zf, eng="scalar")
            P.copy(yt[:, :], ps[2][0:64, 0:HW], eng="scalar")
            y3 = yt[:, :].rearrange("p (h v) -> p h v", v=C)
            P.op("vector", "tensor_reduce", out=st8[0][:, :], in_=y3, axis=AX.X, op=ALU.add)
            P.ts(st8[0][:, :], st8[0][:, :], 1.0 / C, None, op0=ALU.mult, eng="vector")
            yn3 = yn[:, :].rearrange("p (h v) -> p h v", v=C)
            P.tt(yn3, y3, st8[0][:, :].unsqueeze(2).broadcast_to([64, NH, C]), ALU.subtract, eng="vector")
            P.tt(yt[:, :], yn[:, :], yn[:, :], ALU.mult, eng="gpsimd")
            P.op("vector", "tensor_reduce", out=st8[1][:, :], in_=y3, axis=AX.X, op=ALU.add)
            P.ts(st8[1][:, :], st8[1][:, :], 1.0 / C, 64e-5, op0=ALU.mult, op1=ALU.add, eng="vector")
            P.act(st8[1][:, :], st8[1][:, :], AF.Sqrt)
            P.op("vector", "reciprocal", out=st8[1][:, :], in_=st8[1][:, :])
            P.tt(yn3, yn3, st8[1][:, :].unsqueeze(2).broadcast_to([64, NH, C]), ALU.mult, eng="vector")
            for cc in range(8):
                P.tr(ps[4][0:64, cc * C:(cc + 1) * C], yn[:, cc * 64:(cc + 1) * 64], cst[0:64, O_IDENT:O_IDENT + 64])
            for cc in range(8):
                o_ = tmpf[cc][0:64, 0:C]
                P.ts(o_, ps[4][0:64, cc * C:(cc + 1) * C], par(P_LNW, cc), par(P_LNB, cc), op0=ALU.mult, op1=ALU.add,
                     eng="vector")
                P.tt(o_, o_, f32a["bon"][:, cc, cs], ALU.add, eng="gpsimd")
                P.tt(moS[:, cc, cs], o_, f32a["gT"][:, cc, cs], ALU.mult, eng="gpsimd")
        if stage < 5:
            P.memset(moS[:, :, :], 0.0)
        outs.append(P.dma("gpsimd", mo3[:, :, t0:t0 + T], moS[:, :, :]))
    P.finish(outs)
    return nc, P


MT = 256
MC_ID = 0
MC_TRI = 128
MC_PSW = 256
MC_INVF = 320
MC_SGN = 321
MC_W = 322
ROPE_THETA = 10000.0
EPS_ = 1e-6


def mla_consts():
    c = np.zeros((128, MC_W), np.float32)
    c[:, MC_ID:MC_ID + 128] = np.eye(128, dtype=np.float32)
    k = np.arange(128)[:, None]
    q = np.arange(128)[None, :]
    c[:, MC_TRI:MC_TRI + 128] = (k <= q).astype(np.float32)
    psw = np.zeros((64, 64), np.float32)
    for i in range(32):
        psw[i + 32, i] = 1.0
        psw[i, i + 32] = 1.0
    c[0:64, MC_PSW:MC_PSW + 64] = psw
    invf = (1.0 / (ROPE_THETA ** (np.arange(0, 64, 2, dtype=np.float32) / np.float32(64)))).astype(np.float32)
    c[0:64, MC_INVF] = np.concatenate([invf, invf])
    c[0:32, MC_SGN] = 1.0
    c[32:64, MC_SGN] = -1.0
    return c


def build_mla(S):
    nc = bass.Bass("TRN2", target_bir_lowering=False)
    T = MT

    def din(name, shape, dt=F32):
        return nc.dram_tensor(name, list(shape), dt, kind="ExternalInput").ap()
    xnT = din("xnT", [D, S], BF16)
    w_cq = din("w_cq", [D, 512]); w_ckv = din("w_ckv", [D, 512]); w_kr = din("w_kr", [D, 64])
    w_uq = din("w_uq", [512, 384]); w_ukv = din("w_ukv", [512, 512])
    nrm = din("nrm", [128, 8])
    posd = din("pos", [1, S], I32)
    cstd = din("mcst", [128, MC_W])
    moT = nc.dram_tensor("moT", [256, S], BF16, kind="ExternalOutput").ap()
    P = Prog(nc)
    cx = Ctx(P, T=T, wcols=256, wk=4)
    pS = [P.ps("pS%d" % i, [128, 512], F32) for i in range(2)]
    po = [P.ps("po%d" % i, [128, 512], F32) for i in range(2)]
    cst = P.sb("mcst_s", [128, MC_W]); P.dma("gpsimd", cst[:], cstd)
    nr = P.sb("nrm_s", [128, 8]); P.dma("gpsimd", nr[:], nrm)
    ident = cst[:, MC_ID:MC_ID + 128]
    trib = P.sb("trib", [128, 128], BF16)
    P.copy(trib[:, :], cst[:, MC_TRI:MC_TRI + 128])
    xa = P.sb("xa", [128, NKC, T], BF16)
    cq = P.sb("cq", [128, 4, T], F32)
    cqb = P.sb("cqb", [128, 4, T], BF16)
    sq = [P.sb("sq%d" % i, [128, T], BF16) for i in range(2)]
    rstd = P.sb("rstd", [128, T], F32)
    qnT = [P.sb("qnT%d" % h, [128, T], BF16) for h in range(2)]
    qrT = [P.sb("qrT%d" % h, [64, T], BF16) for h in range(2)]
    xr = P.sb("xr", [64, T], F32)
    rt1 = P.sb("rt1", [64, T], F32)
    rt2 = P.sb("rt2", [64, T], F32)
    posi = P.sb("posi", [64, T], I32)
    ang = P.sb("ang", [64, T], F32)
    CF = P.sb("CF", [64, T], F32)
    SF = P.sb("SF", [64, T], F32)
    vT = P.sb("vT", [128, T], F32)
    KnT = P.sb("KnT", [128, 2, S], BF16)
    KrT = P.sb("KrT", [64, S], BF16)
    NT = S // 128
    Vx = P.sb("Vx", [128, 2, NT, 130], BF16)
    P.memset(Vx[:, :, :, 128:130], 1.0)
    pt = [P.sb("pt%d" % i, [128, T], BF16) for i in range(2)]
    rec = P.sb("rec", [128, 2], F32)
    otok = [P.sb("otok%d" % i, [128, 128], F32) for i in range(2)]
    moS = P.sb("moS", [128, 2, T], BF16)
    outs = []
    xn3 = xnT.rearrange("(k p) t -> p k t", p=128)
    mo3 = moT.rearrange("(h p) t -> p h t", p=128)
    scale = 192.0 ** -0.5
    TWO_PI = 2.0 * math.pi

    def rope(dst):
        bank = cx.pl[0]
        P.mm(bank[0:64, 0:T], cst[0:64, MC_PSW:MC_PSW + 64], xr[:, :])
        P.tt(rt1[:, :], xr[:, :], CF[:, :], ALU.mult, eng="gpsimd")
        P.tt(rt2[:, :], bank[0:64, 0:T], SF[:, :], ALU.mult, eng="vector")
        P.tt(dst, rt1[:, :], rt2[:, :], ALU.add, eng="vector")

    for b in range(S // T):
        t0 = b * T
        bs = slice(t0, t0 + T)
        P.dma("gpsimd", xa[:, 0:8, :], xn3[:, 0:8, bs])
        P.dma("gpsimd", xa[:, 8:16, :], xn3[:, 8:16, bs])
        P.dma("gpsimd", posi[:, :], posd[0:1, bs].broadcast_to([64, T]))
        P.copy(ang[:, :], posi[:, :], eng="vector")
        P.ts(ang[:, :], ang[:, :], cst[0:64, MC_INVF:MC_INVF + 1], None, op0=ALU.mult)
        C1 = 6.28125
        C2 = TWO_PI - C1
        P.ts(rt1[:, :], ang[:, :], 1.0 / TWO_PI, None, op0=ALU.mult)
        P.copy(posi[:, :], rt1[:, :], eng="vector")
        P.copy(rt1[:, :], posi[:, :], eng="vector")
        P.stt(ang[:, :], rt1[:, :], -C1, ang[:, :], ALU.mult, ALU.add)
        P.stt(ang[:, :], rt1[:, :], -C2, ang[:, :], ALU.mult, ALU.add)

        def wrap(x):
            P.ts(rt2[:, :], x, math.pi, TWO_PI, op0=ALU.is_gt, op1=ALU.mult)
            P.tt(x, x, rt2[:, :], ALU.subtract)
            P.ts(rt2[:, :], x, -math.pi, TWO_PI, op0=ALU.is_lt, op1=ALU.mult)
            P.tt(x, x, rt2[:, :], ALU.add)
        wrap(ang[:, :])
        P.act(SF[:, :], ang[:, :], AF.Sin)
        P.ts(SF[:, :], SF[:, :], cst[0:64, MC_SGN:MC_SGN + 1], -1.0, op0=ALU.mult, op1=ALU.mult)
        P.ts(rt1[:, :], ang[:, :], 0.5 * math.pi, None, op0=ALU.add)
        wrap(rt1[:, :])
        P.act(CF[:, :], rt1[:, :], AF.Sin)

        def lora(wdn, goff):
            linear(cx, wdn, full_k(D), full_segs(0, 512), lambda k: xa[:, k, :],
                   lambda si, psu: P.copy(cq[:, si, :], psu, eng="scalar"))
            rms_stats(cx, po[0], lambda c: cq[:, c, :], 4, sq, 512, EPS_, rstd[:, :])
            for c in range(4):
                P.stt(cqb[:, c, :], cq[:, c, :], nr[:, goff + c:goff + c + 1], rstd[:, :], ALU.mult, ALU.mult)
        lora(w_cq, 0)
        for h in range(2):
            def cons_q(si, psu, h=h):
                if si == 0:
                    P.copy(qnT[h][:, :], psu, eng="scalar")
                else:
                    P.copy(xr[:, :], psu, eng="scalar")
                    rope(qrT[h][:, :])
            linear(cx, w_uq, full_k(512), [(h * 192, 128), (h * 192 + 128, 64)], lambda k: cqb[:, k, :], cons_q)
        def cons_kr(si, psu):
            P.copy(xr[:, :], psu, eng="scalar")
            rope(KrT[:, bs])
        linear(cx, w_kr, full_k(D), [(0, 64)], lambda k: xa[:, k, :], cons_kr)
        lora(w_ckv, 4)
        for h in range(2):
            def cons_kv(si, psu, h=h):
                if si == 0:
                    P.copy(KnT[:, h, bs], psu, eng="scalar")
                else:
                    P.copy(vT[:, :], psu, eng="scalar")
                    for j in range(2):
                        bank = cx.pl[1 + j]
                        P.tr(bank[:, 0:128], vT[:, j * 128:(j + 1) * 128], ident)
                        P.copy(Vx[:, h, 2 * b + j, 0:128], bank[:, 0:128], eng="vector")
            linear(cx, w_ukv, full_k(512), [(h * 256, 128), (h * 256 + 128, 128)], lambda k: cqb[:, k, :], cons_kv)
        for h in range(2):
            nkt = 2 * b + 2
            for kt in range(nkt):
                ks = slice(kt * 128, (kt + 1) * 128)
                bank = pS[kt % 2]
                P.mm(bank[:, 0:T], KnT[:, h, ks], qnT[h][:, :], start=True, stop=False)
                P.mm(bank[:, 0:T], KrT[:, ks], qrT[h][:, :], start=False, stop=True)
                p_ = pt[kt % 2]
                P.act(p_[:, :], bank[:, 0:T], AF.Exp, scale=scale)
                for j in range(2):
                    qt = 2 * b + j
                    if kt > qt:
                        continue
                    js = slice(j * 128, (j + 1) * 128)
                    if kt == qt:
                        P.tt(p_[:, js], p_[:, js], trib[:, :], ALU.mult, eng="vector")
                    P.mm(po[j][:, 0:129], p_[:, js], Vx[:, h, kt, 0:129], start=(kt == 0), stop=(kt == qt))
            for j in range(2):
                P.op("vector", "reciprocal", out=rec[:, j:j + 1], in_=po[j][:, 128:129])
                P.ts(otok[j][:, :], po[j][:, 0:128], rec[:, j:j + 1], None, op0=ALU.mult)
                bank = cx.pl[3]
                P.tr(bank[:, 0:128], otok[j][:, :], ident)
                P.copy(moS[:, h, j * 128:(j + 1) * 128], bank[:, 0:128], eng="scalar")
        outs.append(P.dma("gpsimd", mo3[:, :, bs], moS[:, :, :]))
    P.finish(outs)
    return nc, P


GT = 256
GC_ID = 0
GC_ML = 128
GC_MU = 192
GC_ID8 = 256
GC_RESET = 768
GC_ONES = GC_RESET + GT
GC_W = GC_ONES + 128
NEG = -30000.0


def gdn_consts():
    c = np.zeros((128, GC_W), np.float32)
    c[:, GC_ID:GC_ID + 128] = np.eye(128, dtype=np.float32)
    i = np.arange(64)[:, None]
    j = np.arange(64)[None, :]
    c[0:64, GC_ML:GC_ML + 64] = np.where(j < i, 0.0, NEG)
    c[0:64, GC_MU:GC_MU + 64] = np.where(j >= i, 0.0, NEG)
    c[0:64, GC_ID8:GC_ID8 + 512] = np.tile(np.eye(64, dtype=np.float32), (1, 8))
    r = np.ones(GT, np.float32)
    r[::64] = 0.0
    c[0, GC_RESET:GC_RESET + GT] = r
    c[0, GC_ONES:GC_ONES + 128] = 1.0
    return c


def gdn_prep(gi, xT, w_in, conv_w, a_log, dt_bias, out_norm):
    h0 = 2 * gi
    cw = np.zeros((128, 3, 2, 4), np.float32)
    for w in range(3):
        for hh in range(2):
            base = w * 1024 + (h0 + hh) * 128
            cw[:, w, hh, :] = conv_w[:, base:base + 128].T
    wba = np.stack([w_in[:, 5184 + h0], w_in[:, 5184 + h0 + 1], w_in[:, 5192 + h0], w_in[:, 5192 + h0 + 1]], axis=1)
    hp = np.array([[a_log[h0], a_log[h0 + 1], dt_bias[h0], dt_bias[h0 + 1]]], np.float32)
    return {"xnT": xT, "wq": w_in[:, 1088 + h0 * 128:1088 + h0 * 128 + 256],
            "wk": w_in[:, 2112 + h0 * 128:2112 + h0 * 128 + 256],
            "wv": w_in[:, 3136 + h0 * 128:3136 + h0 * 128 + 256],
            "wz": w_in[:, 4160 + h0 * 128:4160 + h0 * 128 + 256],
            "wba": wba, "cw": cw.reshape(128, 24), "hp": hp,
            "onorm": np.asarray(out_norm, np.float32).reshape(128, 1), "gcst": gdn_consts()}


def build_gdn(S):
    nc = bass.Bass("TRN2", target_bir_lowering=False)
    T = GT
    NCK = T // 64

    def din(name, shape, dt=F32):
        return nc.dram_tensor(name, list(shape), dt, kind="ExternalInput").ap()
    xnT = din("xnT", [D, S], BF16)
    wq = din("wq", [D, 256]); wk = din("wk", [D, 256]); wv = din("wv", [D, 256]); wz = din("wz", [D, 256])
    wba = din("wba", [D, 4])
    cwd = din("cw", [128, 24]); hpd = din("hp", [1, 4]); ond = din("onorm", [128, 1])
    cstd = din("gcst", [128, GC_W])
    moT = nc.dram_tensor("moT", [256, S], BF16, kind="ExternalOutput").ap()
    P = Prog(nc)
    cx = Ctx(P, T=T, wcols=256, wk=4)
    pX, pE, pO, pZ = [P.ps("pg%d" % i, [128, 512], F32) for i in range(4)]
    pl = cx.pl
    cst = P.sb("gcst_s", [128, GC_W]); P.dma("gpsimd", cst[:], cstd)
    cw = P.sb("cw_s", [128, 24]); P.dma("gpsimd", cw[:], cwd)
    hp = P.sb("hp_s", [1, 4]); P.dma("gpsimd", hp[:], hpd)
    onr = P.sb("on_s", [128, 1]); P.dma("gpsimd", onr[:], ond)
    ident = cst[:, GC_ID:GC_ID + 128]
    id64 = cst[0:64, GC_ID:GC_ID + 64]
    id64b = P.sb("id64b", [64, 64], BF16); P.copy(id64b[:, :], id64)
    ones_row = cst[0:1, GC_ONES:GC_ONES + 128]
    one11 = cst[0:1, GC_ONES:GC_ONES + 1]
    reset = cst[0:1, GC_RESET:GC_RESET + T]
    nA = P.sb("nA", [1, 2], F32)
    P.act(nA[:, :], hp[0:1, 0:2], AF.Exp)
    P.ts(nA[:, :], nA[:, :], -1.0, None, op0=ALU.mult)

    xa = P.sb("xa", [128, NKC, T], BF16)
    raw = P.sb("raw", [128, 3, 2, T + 3], F32)
    P.memset(raw[:], 0.0)
    szT = P.sb("szT", [128, 2, T], F32)
    rows = {n: P.sb("r_" + n, [1, 2, T], F32) for n in ("b", "g", "gam", "eg", "ebg", "ed", "t")}
    bc = {n: P.sb("bc_" + n, [128, 2, T], F32) for n in ("eg", "ebg", "ed", "b", "gam")}
    cols = P.sb("cols", [64, 16], F32)
    ncols = P.sb("ncols", [64, 16], F32)
    cv = P.sb("cv", [128, 3, 2, T], F32)
    acc = [P.sb("acc%d" % i, [128, T], F32) for i in range(2)]
    sqb = [P.sb("sqb%d" % i, [128, T], BF16) for i in range(2)]
    rs_ = P.sb("rs_", [128, T], F32)
    qn = P.sb("qn", [128, 2, T], F32); kn = P.sb("kn", [128, 2, T], F32)
    qnb = P.sb("qnb", [128, 2, T], BF16); knb = P.sb("knb", [128, 2, T], BF16)
    qd = P.sb("qd", [128, 2, T], BF16); nkbg = P.sb("nkbg", [128, 2, T], BF16)
    kdF = P.sb("kdF", [128, 2, T], F32); vbF = P.sb("vbF", [128, 2, T], F32)
    kdt = P.sb("kdt", [64, 8, 128], BF16); vbt = P.sb("vbt", [64, 8, 128], BF16)
    dx = P.sb("dx", [64, 4, 64], F32)
    Dl = P.sb("Dl", [64, 8, 64], F32); Du = P.sb("Du", [64, 8, 64], F32)
    P0f = P.sb("P0f", [64, 8, 64], F32)
    attnT = P.sb("attnT", [64, 512], BF16)
    pwt = {n: P.sb("g" + n, [64, 512], BF16) for n in ("Pa", "PaT", "Pb", "PbT", "TT")}
    Sst = P.sb("Sst", [128, 2, 128], F32); Sb = P.sb("Sb", [128, 2, 128], BF16)
    P.memset(Sst[:], 0.0); P.memset(Sb[:], 0.0)
    x1 = P.sb("x1", [64, 256], BF16); et = P.sb("et", [64, 256], BF16)
    ot = P.sb("ot", [64, 256], F32); osq = P.sb("osq", [64, 256], F32); on = P.sb("on", [64, 256], F32)
    st2 = P.sb("st2", [64, 2], F32)
    moS = P.sb("moS", [128, 2, T], BF16)
    outs = []
    xn3 = xnT.rearrange("(k p) t -> p k t", p=128)
    mo3 = moT.rearrange("(h p) t -> p h t", p=128)

    def cwc(w, hh, j):
        i = (w * 2 + hh) * 4 + j
        return cw[:, i:i + 1]

    for b in range(S // T):
        t0 = b * T
        bs = slice(t0, t0 + T)
        P.dma("gpsimd", xa[:, 0:8, :], xn3[:, 0:8, bs])
        P.dma("gpsimd", xa[:, 8:16, :], xn3[:, 8:16, bs])
        if b > 0:
            for w in range(3):
                P.copy(raw[:, w, :, 0:3], raw[:, w, :, T:T + 3], eng="gpsimd")
        two = [(0, 128), (128, 128)]
        for w, W in enumerate((wq, wk, wv)):
            linear(cx, W, full_k(D), two, lambda k: xa[:, k, :],
                   lambda si, psu, w=w: P.copy(raw[:, w, si, 3:T + 3], psu, eng="scalar"))
        linear(cx, wz, full_k(D), two, lambda k: xa[:, k, :],
               lambda si, psu: P.act(szT[:, si, :], psu, AF.Silu))

        def cons_ba(si, psu):
            if si < 2:
                P.act(rows["b"][0:1, si, :], psu, AF.Sigmoid)
            else:
                hh = si - 2
                t = rows["t"][0:1, hh, :]
                P.act(t, psu, AF.Exp, bias=hp[0:1, 2 + hh:3 + hh])
                P.ts(t, t, 1.0, None, op0=ALU.add)
                P.act(t, t, AF.Ln)
                P.ts(rows["g"][0:1, hh, :], t, nA[0:1, hh:hh + 1], None, op0=ALU.mult)
        linear(cx, wba, full_k(D), [(0, 1), (1, 1), (2, 1), (3, 1)], lambda k: xa[:, k, :], cons_ba)
        for hh in range(2):
            P.op("vector", "tensor_tensor_scan", out=rows["gam"][0:1, hh, :], data0=reset, data1=rows["g"][0:1, hh, :],
                 initial=0.0, op0=ALU.mult, op1=ALU.add)
        P.act(rows["eg"][:, :, :], rows["gam"][:, :, :], AF.Exp)
        P.tt(rows["ebg"][:, :, :], rows["eg"][:, :, :], rows["b"][:, :, :], ALU.mult)
        g8 = rows["gam"][:, :, :].rearrange("p h (c t) -> p (h c) t", t=64)
        P.tt(rows["ed"][:, :, :].rearrange("p h (c t) -> p (h c) t", t=64), g8[:, :, 63:64].broadcast_to([1, 8, 64]), g8,
             ALU.subtract)
        P.act(rows["ed"][:, :, :], rows["ed"][:, :, :], AF.Exp)
        for i, n in enumerate(("eg", "ebg", "ed", "b", "gam")):
            bank = pl[i % 4]
            P.mm(bank[:, 0:2 * T], ones_row, rows[n][0:1, :, :].rearrange("p h t -> p (h t)"))
            P.copy(bc[n][:, :, :].rearrange("p h t -> p (h t)"), bank[:, 0:2 * T], eng="scalar" if i % 2 else "vector")
        for hh in range(2):
            for c in range(NCK):
                s = hh * 4 + c
                P.mm(pX[0:64, s:s + 1], rows["gam"][0:1, hh, c * 64:(c + 1) * 64], one11)
                P.mm(pX[0:64, 8 + s:9 + s], rows["b"][0:1, hh, c * 64:(c + 1) * 64], one11)
        P.copy(cols[:, :], pX[0:64, 0:16], eng="vector")
        P.ts(ncols[:, :], cols[:, :], -1.0, None, op0=ALU.mult)
        for w in range(3):
            for hh in range(2):
                a_ = acc[(w * 2 + hh) % 2]
                P.ts(a_[:, :], raw[:, w, hh, 0:T], cwc(w, hh, 0), None, op0=ALU.mult)
                for j in range(1, 4):
                    P.stt(a_[:, :], raw[:, w, hh, j:j + T], cwc(w, hh, j), a_[:, :], ALU.mult, ALU.add)
                P.act(cv[:, w, hh, :], a_[:, :], AF.Silu)
        for w, dst, dstb, scl in ((0, qn, qnb, 128.0 ** -0.5), (1, kn, knb, 1.0)):
            for hh in range(2):
                s_ = sqb[hh]
                P.act(s_[:, :], cv[:, w, hh, :], AF.Square)
                bank = pl[(w * 2 + hh) % 4]
                P.mm(bank[:, 0:T], cx.ones[:, :], s_[:, :])
                P.ts(rs_[:, :], bank[:, 0:T], 1e-6, None, op0=ALU.add)
                P.act(rs_[:, :], rs_[:, :], AF.Sqrt)
                P.op("vector", "reciprocal", out=rs_[:, :], in_=rs_[:, :])
                P.stt(dst[:, hh, :], cv[:, w, hh, :], scl, rs_[:, :], ALU.mult, ALU.mult)
                P.copy(dstb[:, hh, :], dst[:, hh, :], eng="gpsimd")
        for hh in range(2):
            P.tt(qd[:, hh, :], qn[:, hh, :], bc["eg"][:, hh, :], ALU.mult, eng="gpsimd")
            P.stt(nkbg[:, hh, :], kn[:, hh, :], -1.0, bc["ebg"][:, hh, :], ALU.mult, ALU.mult)
            P.tt(kdF[:, hh, :], kn[:, hh, :], bc["ed"][:, hh, :], ALU.mult, eng="gpsimd")
            P.tt(vbF[:, hh, :], cv[:, 2, hh, :], bc["b"][:, hh, :], ALU.mult, eng="gpsimd")
        for srcF, dstT, bank in ((kdF, kdt, pl[0]), (vbF, vbt, pl[1])):
            for hh in range(2):
                for c in range(NCK):
                    P.tr(bank[0:64, c * 128:(c + 1) * 128], srcF[:, hh, c * 64:(c + 1) * 64], ident)
                P.copy(dstT[:, hh * 4:(hh + 1) * 4, :].rearrange("p s v -> p (s v)"), bank[0:64, 0:512],
                       eng="scalar" if hh else "vector")
        for hh in range(2):
            sl = slice(hh * 4, hh * 4 + 4)
            gb3 = bc["gam"][0:64, hh, :].rearrange("p (c t) -> p c t", t=64)
            gc = cols[:, sl].unsqueeze(2).broadcast_to([64, 4, 64])
            P.tt(dx[:, :, :], gc, gb3, ALU.subtract)
            P.tt(dx[:, :, :], dx[:, :, :], cst[0:64, GC_ML:GC_ML + 64].unsqueeze(1).broadcast_to([64, 4, 64]), ALU.add)
            P.act(Dl[:, sl, :], dx[:, :, :], AF.Exp)
            P.tt(dx[:, :, :], gb3, gc, ALU.subtract)
            P.tt(dx[:, :, :], dx[:, :, :], cst[0:64, GC_MU:GC_MU + 64].unsqueeze(1).broadcast_to([64, 4, 64]), ALU.add)
            P.act(Du[:, sl, :], dx[:, :, :], AF.Exp)
        for hh in range(2):
            for c in range(NCK):
                s = hh * 4 + c
                cs = slice(c * 64, (c + 1) * 64)
                P.mm(pl[2][0:64, s * 64:(s + 1) * 64], knb[:, hh, cs], knb[:, hh, cs])
                P.mm(pl[3][0:64, s * 64:(s + 1) * 64], knb[:, hh, cs], qnb[:, hh, cs])
        P.tt(P0f[:, :, :], pl[2][0:64, 0:512].rearrange("p (s t) -> p s t", t=64), Dl[:, :, :], ALU.mult)
        P.tt(P0f[:, :, :], P0f[:, :, :], ncols[:, 8:16].unsqueeze(2).broadcast_to([64, 8, 64]), ALU.mult)
        P.tt(attnT[:, :].rearrange("p (s t) -> p s t", t=64), pl[3][0:64, 0:512].rearrange("p (s t) -> p s t", t=64),
             Du[:, :, :], ALU.mult)
        P.copy(pwt["Pa"][:, :], P0f[:, :, :].rearrange("p s t -> p (s t)"), eng="gpsimd")
        for s in range(8):
            P.tr(pl[0][0:64, s * 64:(s + 1) * 64], P0f[:, s, :], id64)
        P.copy(pwt["PaT"][:, :], pl[0][0:64, 0:512], eng="scalar")
        P.tt(pwt["TT"][:, :], pwt["PaT"][:, :], cst[0:64, GC_ID8:GC_ID8 + 512], ALU.add, eng="gpsimd")
        cur = ("Pa", "PaT")
        bk = (pX, pE, pO)
        for lvl in range(1, 6):
            pn, pnt = cur
            nn, nnt = ("Pb", "PbT") if pn == "Pa" else ("Pa", "PaT")
            for s in range(8):
                hs = slice(s * 64, (s + 1) * 64)
                P.mm(bk[0][0:64, hs], pwt[pnt][:, hs], pwt[pn][:, hs])
            if lvl < 5:
                for s in range(8):
                    hs = slice(s * 64, (s + 1) * 64)
                    P.mm(bk[1][0:64, hs], pwt[pn][:, hs], pwt[pnt][:, hs])
            P.copy(pwt[nn][:, :], bk[0][0:64, 0:512], eng="scalar")
            if lvl < 5:
                P.copy(pwt[nnt][:, :], bk[1][0:64, 0:512], eng="vector")
            for s in range(8):
                hs = slice(s * 64, (s + 1) * 64)
                P.mm(bk[2][0:64, hs], pwt[nn][:, hs], pwt["TT"][:, hs])
            P.tt(pwt["TT"][:, :], pwt["TT"][:, :], bk[2][0:64, 0:512], ALU.add, eng="vector")
            cur = (nn, nnt)
        for c in range(NCK):
            cs = slice(c * 64, (c + 1) * 64)
            for hh in range(2):
                s = hh * 4 + c
                vs = slice(hh * 128, (hh + 1) * 128)
                P.mm(pX[0:64, vs], nkbg[:, hh, cs], Sb[:, hh, :], start=True, stop=False)
                P.mm(pX[0:64, vs], id64b[:, :], vbt[:, s, :], start=False, stop=True)
            P.copy(x1[:, :], pX[0:64, 0:256], eng="scalar")
            for hh in range(2):
                s = hh * 4 + c
                vs = slice(hh * 128, (hh + 1) * 128)
                P.mm(pE[0:64, vs], pwt["TT"][:, s * 64:(s + 1) * 64], x1[:, vs])
            P.copy(et[:, :], pE[0:64, 0:256], eng="vector")
            for hh in range(2):
                s = hh * 4 + c
                vs = slice(hh * 128, (hh + 1) * 128)
                P.mm(pO[0:64, vs], qd[:, hh, cs], Sb[:, hh, :], start=True, stop=False)
                P.mm(pO[0:64, vs], attnT[:, s * 64:(s + 1) * 64], et[:, vs], start=False, stop=True)
                P.mm(pZ[:, vs], kdt[:, s, :], et[:, vs])
            for hh in range(2):
                vs = slice(hh * 128, (hh + 1) * 128)
                last = bc["eg"][:, hh, c * 64 + 63:c * 64 + 64]
                P.stt(Sst[:, hh, :], Sst[:, hh, :], last, pZ[:, vs], ALU.mult, ALU.add)
            P.copy(Sb[:, :, :], Sst[:, :, :], eng="scalar")
            P.copy(ot[:, :], pO[0:64, 0:256], eng="scalar")
            P.tt(osq[:, :], ot[:, :], ot[:, :], ALU.mult, eng="gpsimd")
            P.op("vector", "tensor_reduce", out=st2[:, :], in_=osq[:, :].rearrange("p (h v) -> p h v", v=128), axis=AX.X,
                 op=ALU.add)
            P.ts(st2[:, :], st2[:, :], 1.0 / 128.0, 1e-6, op0=ALU.mult, op1=ALU.add)
            P.act(st2[:, :], st2[:, :], AF.Sqrt)
            P.op("vector", "reciprocal", out=st2[:, :], in_=st2[:, :])
            P.tt(on[:, :].rearrange("p (h v) -> p h v", v=128), ot[:, :].rearrange("p (h v) -> p h v", v=128),
                 st2[:, :].unsqueeze(2).broadcast_to([64, 2, 128]), ALU.mult)
            for hh in range(2):
                P.tr(pl[1][:, hh * 64:(hh + 1) * 64], on[:, hh * 128:(hh + 1) * 128], id64)
            for hh in range(2):
                P.stt(moS[:, hh, cs], pl[1][:, hh * 64:(hh + 1) * 64], onr[:, 0:1], szT[:, hh, cs], ALU.mult, ALU.mult)
        outs.append(P.dma("gpsimd", mo3[:, :, bs], moS[:, :, :]))
    P.finish(outs)
    return nc, P


def _run(nc, maps):
    maps = [{k: np.ascontiguousarray(v) for k, v in m.items()} for m in maps]
    return run_bass_kernel_spmd(nc, maps, core_ids=list(range(len(maps)))).results


def _gl(gg):
    gg = np.asarray(gg, np.float32)
    return np.ascontiguousarray(gg.reshape(-1, 16, 128).transpose(2, 0, 1).reshape(128, -1))


def kernel(**inp):
    A = lambda n: np.asarray(inp[n])
    x = np.asarray(inp["x"], np.float32)
    B, S, _ = x.shape
    NT = B * S
    NG = 4
    hT = np.ascontiguousarray(x.reshape(NT, D).T)
    nc, _p = build_first(NT)
    xnT = _run(nc, [{"hT": hT, "gains": _gl(A("norm_mix_pre")[0:1])}])[0]["xnTo"]
    positions = np.asarray(inp["positions"]).astype(np.int32)
    vfirst = {}
    for l in range(4):
        moT = np.zeros((D, NT), xnT.dtype)
        cores = [(b, g) for b in range(B) for g in range(NG)]
        if l % 2 == 0:
            e = l // 2
            w_in = A("hyb_w_in")[e]
            uq = A("mla_w_uq")[e]
            ukv = A("mla_w_ukv")[e]
            nrm = np.concatenate([A("mla_q_norm")[e].reshape(4, 128).T, A("mla_kv_norm")[e].reshape(4, 128).T], axis=1)
            mcst = mla_consts()
            maps = []
            for b, g in cores:
                maps.append({"xnT": xnT[:, b * S:(b + 1) * S], "w_cq": w_in[:, 0:512], "w_ckv": w_in[:, 512:1024],
                             "w_kr": w_in[:, 1024:1088], "w_uq": uq[:, g * 384:(g + 1) * 384],
                             "w_ukv": ukv[:, g * 512:(g + 1) * 512], "nrm": nrm.astype(np.float32),
                             "pos": positions[b:b + 1, :], "mcst": mcst})
            nc, _p = build_mla(S)
            res = _run(nc, maps)
            for i, (b, g) in enumerate(cores):
                moT[g * 256:(g + 1) * 256, b * S:(b + 1) * S] = res[i]["moT"]
            maps = []
            for b, g in cores:
                maps.append(gdn_prep(g, xnT[:, b * S:(b + 1) * S], w_in, A("gdn_conv_w")[e], A("gdn_a_log")[e],
                                     A("gdn_dt_bias")[e], A("gdn_out_norm")[e]))
            nc, _p = build_gdn(S)
            res = _run(nc, maps)
            for i, (b, g) in enumerate(cores):
                moT[1024 + g * 256:1024 + (g + 1) * 256, b * S:(b + 1) * S] = res[i]["moT"]
            w_o = A("hyb_w_out")[e]
        else:
            o = l // 2
            vres = o > 0
            R_ = lambda n: A(n)[o]
            mix = np.ascontiguousarray(R_("rwkv_mix").reshape(6, 16, 128).transpose(2, 0, 1).reshape(128, 96))
            cstv = rwkv_consts()
            maps = []
            for b, g in cores:
                cs = slice(g * CH, (g + 1) * CH)
                pl_ = lambda v: np.asarray(v, np.float32).reshape(-1)[cs].reshape(8, 64).T
                chp = np.concatenate([pl_(R_("rwkv_w0")), pl_(R_("rwkv_a0")), pl_(R_("rwkv_k_k")), pl_(R_("rwkv_k_a")),
                                      pl_(R_("rwkv_r_k")), pl_(R_("rwkv_ln_w")), pl_(R_("rwkv_ln_b")),
                                      pl_(A("rwkv_v0")[o - 1]) if vres else np.zeros((64, 8), np.float32)],
                                     axis=1).astype(np.float32)
                xT = np.zeros((D, S + 1), xnT.dtype)
                xT[:, 1:] = xnT[:, b * S:(b + 1) * S]
                m = {"xnT": xT, "w_r": R_("rwkv_w_r")[:, cs], "w_k": R_("rwkv_w_k")[:, cs], "w_v": R_("rwkv_w_v")[:, cs],
                     "w1": R_("rwkv_w1"), "w2": R_("rwkv_w2")[:, cs], "a1": R_("rwkv_a1"), "a2": R_("rwkv_a2")[:, cs],
                     "g1": R_("rwkv_g1"), "g2": R_("rwkv_g2")[:, cs], "mix": mix, "chp": chp, "cst": cstv}
                if vres:
                    m.update({"v1": A("rwkv_v1")[o - 1], "v2": A("rwkv_v2")[o - 1][:, cs], "vfT": vfirst[(b, g)]})
                maps.append(m)
            nc, _p = build_rwkv(S, vres)
            res = _run(nc, maps)
            for i, (b, g) in enumerate(cores):
                moT[g * CH:(g + 1) * CH, b * S:(b + 1) * S] = res[i]["moT"]
                if not vres:
                    vfirst[(b, g)] = res[i]["voT"]
            w_o = A("rwkv_w_o")[o]
        gains = _gl(np.stack([A("norm_mix_post")[l], A("norm_ffn_pre")[l], A("norm_ffn_post")[l],
                              A("norm_mix_pre")[min(l + 1, 3)]]))
        nct = 4 if (NT // 4) % TB == 0 else (2 if (NT // 2) % TB == 0 else 1)
        n = NT // nct
        nc, _p = build_tl(n)
        rr = _run(nc, [{"hT": hT[:, c * n:(c + 1) * n], "moT": moT[:, c * n:(c + 1) * n], "wo": w_o,
                        "wg": A("ffn_w_gate")[l], "wu": A("ffn_w_up")[l], "wd": A("ffn_w_down")[l], "gains": gains}
                       for c in range(nct)])
        hT = np.concatenate([r["hTo"] for r in rr], axis=1)
        xnT = np.concatenate([r["xnTo"] for r in rr], axis=1)
    return np.ascontiguousarray(np.asarray(hT, np.float32).T).reshape(B, S, D)
```
